# Optimizing a Trainium2 kernel written in Bass

```python
import jax, jax.numpy as jnp
from jax import lax
import numpy as np

D_MODEL = 1024
BATCH = 16
SEQ = 2048
DEPTH = 2

HEAD_DIM = 64
N_HEADS_A = 8
N_HEADS_B = 8
D_A = N_HEADS_A * HEAD_DIM
D_B = N_HEADS_B * HEAD_DIM
D_MIX = D_A + D_B
D_IN = 4 * D_A + 4 * D_B
SPLIT_POINTS = (D_A, 2 * D_A, 3 * D_A, 4 * D_A,
                4 * D_A + D_B, 4 * D_A + 2 * D_B, 4 * D_A + 3 * D_B)
DILATED_PATTERNS = ((128, 1), (512, 4), (2048, 16))
MOBA_BLOCK = 256
MOBA_TOPK = 3
MOBA_QCHUNK = 128
ROPE_THETA = 500000.0
ROPE_DIM = HEAD_DIM // 4
RMS_EPS = 1e-6
NEG_INF = -1e30

kernel_name = "hymba_dilated_moba_sandwich_adaln"


def rms_norm(x, g):
    x32 = x.astype(jnp.float32)
    y = x32 * lax.rsqrt(jnp.mean(x32 * x32, axis=-1, keepdims=True) + RMS_EPS)
    return (y * g.astype(jnp.float32)).astype(x.dtype)


def rope_tables(positions):
    inv_freq = ROPE_THETA ** (-jnp.arange(0, ROPE_DIM, 2, dtype=jnp.float32) / ROPE_DIM)
    ang = positions.astype(jnp.float32)[:, None, :, None] * inv_freq
    return jnp.cos(ang), jnp.sin(ang)


def apply_partial_rope(t, cos, sin):
    r = ROPE_DIM // 2
    cos = cos.astype(t.dtype)
    sin = sin.astype(t.dtype)
    t1, t2, rest = t[..., :r], t[..., r:ROPE_DIM], t[..., ROPE_DIM:]
    return jnp.concatenate([t1 * cos - t2 * sin, t2 * cos + t1 * sin, rest], axis=-1)


def dilated_pattern(q, k, v, window, dilation):
    B, H, S, hd = q.shape
    n = window // dilation
    span = n * dilation
    s_pad = -(-S // span) * span
    L = s_pad // dilation

    def strided(t):
        t = jnp.pad(t, ((0, 0), (0, 0), (0, s_pad - S), (0, 0)))
        t = t.reshape(B, H, L, dilation, hd).transpose(0, 1, 3, 2, 4)
        return t.reshape(B, H, dilation, L // n, n, hd)

    qb, kb, vb = strided(q), strided(k), strided(v)
    nblk = L // n

    def with_prev(t):
        prev = jnp.pad(t, ((0, 0), (0, 0), (0, 0), (1, 0), (0, 0), (0, 0)))[:, :, :, :-1]
        return jnp.concatenate([prev, t], axis=4)

    kw, vw = with_prev(kb), with_prev(vb)
    s = jnp.einsum('bhrnqd,bhrnkd->bhrnqk', qb, kw,
                   preferred_element_type=jnp.float32) * (hd ** -0.5)
    qi = jnp.arange(n)[:, None]
    kj = jnp.arange(2 * n)[None, :]
    delta = qi + n - kj
    blk = jnp.arange(nblk)[:, None, None]
    valid = (delta >= 0) & (delta <= n) & (blk * n + kj - n >= 0)
    s = jnp.where(valid, s, NEG_INF)
    m = jnp.max(s, axis=-1, keepdims=True)
    p = jnp.exp(s - m)
    denom = jnp.sum(p, axis=-1)
    num = jnp.einsum('bhrnqk,bhrnkd->bhrnqd', p.astype(v.dtype), vw,
                     preferred_element_type=jnp.float32)
    o = num / denom[..., None]
    lse = m[..., 0] + jnp.log(denom)
    o = o.reshape(B, H, dilation, L, hd).transpose(0, 1, 3, 2, 4).reshape(B, H, s_pad, hd)[:, :, :S]
    lse = lse.reshape(B, H, dilation, L).transpose(0, 1, 3, 2).reshape(B, H, s_pad)[:, :, :S]
    return o, lse


def dilated_mixture_attention(q, k, v):
    outs, lses = [], []
    for window, dilation in DILATED_PATTERNS:
        o, lse = dilated_pattern(q, k, v, window, dilation)
        outs.append(o)
        lses.append(lse)
    alpha = jax.nn.softmax(jnp.stack(lses, axis=0), axis=0)
    o = jnp.einsum('pbhs,pbhsd->bhsd', alpha, jnp.stack(outs, axis=0))
    return o.astype(q.dtype)


def moba_attention(q, k, v):
    B, H, S, hd = q.shape
    blk = MOBA_BLOCK
    s_pad = -(-S // blk) * blk
    nb = s_pad // blk
    padw = ((0, 0), (0, 0), (0, s_pad - S), (0, 0))
    q, k, v = jnp.pad(q, padw), jnp.pad(k, padw), jnp.pad(v, padw)
    kb = k.reshape(B, H, nb, blk, hd)
    vb = v.reshape(B, H, nb, blk, hd)
    scale = hd ** -0.5
    topk = min(MOBA_TOPK, nb)

    kmean = jnp.mean(kb.astype(jnp.float32), axis=3)
    gate = jnp.einsum('bhsd,bhnd->bhsn', q.astype(jnp.float32), kmean)
    own = jnp.arange(s_pad) // blk
    past = jnp.arange(nb)[None, :] < own[:, None]
    gate = jnp.where(past, gate, NEG_INF)
    _, sel = lax.top_k(gate, topk)

    qc = MOBA_QCHUNK
    nchunk = s_pad // qc
    q_chunks = q.reshape(B, H, nchunk, qc, hd)
    sel_chunks = sel.reshape(B, H, nchunk, qc, topk)
    hidx = jnp.arange(H)[:, None, None]

    def body(idx):
        b = idx // nchunk
        c = idx % nchunk
        q_c = q_chunks[b][:, c]
        sel_c = sel_chunks[b][:, c]
        kb_b, vb_b = kb[b], vb[b]
        qpos = c * qc + jnp.arange(qc)
        ob = (c * qc) // blk
        k_own = lax.dynamic_index_in_dim(kb_b, ob, axis=1, keepdims=False)
        v_own = lax.dynamic_index_in_dim(vb_b, ob, axis=1, keepdims=False)
        kpos = ob * blk + jnp.arange(blk)
        s_own = jnp.einsum('hqd,hkd->hqk', q_c, k_own,
                           preferred_element_type=jnp.float32) * scale
        s_own = jnp.where(kpos[None, :] <= qpos[:, None], s_own, NEG_INF)
        k_sel = kb_b[hidx, sel_c]
        v_sel = vb_b[hidx, sel_c]
        s_sel = jnp.einsum('hqd,hqnkd->hqnk', q_c, k_sel,
                           preferred_element_type=jnp.float32) * scale
        valid = jnp.arange(topk)[None, :] < jnp.minimum(qpos // blk, topk)[:, None]
        s_sel = jnp.where(valid[None, :, :, None], s_sel, NEG_INF)
        scores = jnp.concatenate([s_own, s_sel.reshape(H, qc, topk * blk)], axis=-1)
        p = jax.nn.softmax(scores, axis=-1)
        p_own = p[..., :blk].astype(v.dtype)
        p_sel = p[..., blk:].reshape(H, qc, topk, blk).astype(v.dtype)
        o = (jnp.einsum('hqk,hkd->hqd', p_own, v_own, preferred_element_type=jnp.float32)
             + jnp.einsum('hqnk,hqnkd->hqd', p_sel, v_sel, preferred_element_type=jnp.float32))
        return o.astype(q.dtype)

    out = lax.map(body, jnp.arange(B * nchunk))
    out = out.reshape(B, nchunk, H, qc, hd).transpose(0, 2, 1, 3, 4).reshape(B, H, s_pad, hd)
    return out[:, :, :S]


def hybrid_layer(x, c, cos, sin, pre_g, post_g, w_ada, b_ada, w_in, w_out):
    B, S, _ = x.shape
    mod = jnp.einsum('bd,de->be', jax.nn.silu(c), w_ada) + b_ada
    shift, scale, gate = jnp.split(mod, 3, axis=-1)
    h = rms_norm(x, pre_g) * (1 + scale[:, None, :]) + shift[:, None, :]
    proj = jnp.einsum('bsd,de->bse', h, w_in)
    qa, ka, va, ga, qb, kb, vb, gb = jnp.split(proj, SPLIT_POINTS, axis=-1)

    def heads(t, nh):
        return t.reshape(B, S, nh, HEAD_DIM).transpose(0, 2, 1, 3)

    def merge(t):
        return t.transpose(0, 2, 1, 3).reshape(B, S, -1)

    qa = apply_partial_rope(heads(qa, N_HEADS_A), cos, sin)
    ka = apply_partial_rope(heads(ka, N_HEADS_A), cos, sin)
    qb = apply_partial_rope(heads(qb, N_HEADS_B), cos, sin)
    kb = apply_partial_rope(heads(kb, N_HEADS_B), cos, sin)
    o_a = merge(dilated_mixture_attention(qa, ka, heads(va, N_HEADS_A))) * jax.nn.silu(ga)
    o_b = merge(moba_attention(qb, kb, heads(vb, N_HEADS_B))) * jax.nn.silu(gb)
    y = jnp.einsum('bse,ed->bsd', jnp.concatenate([o_a, o_b], axis=-1), w_out)
    y = rms_norm(y, post_g)
    return x + gate[:, None, :] * y


def setup_inputs(seed: int = 0) -> dict:
    key = jax.random.key(seed)
    ks = jax.random.split(key, 11)
    x = jax.random.normal(ks[0], (BATCH, SEQ, D_MODEL), jnp.float32)
    c = jax.random.normal(ks[1], (BATCH, D_MODEL), jnp.float32)
    offset = jax.random.randint(ks[2], (BATCH, 1), 0, 4096, dtype=jnp.int32)
    positions = (jnp.arange(SEQ, dtype=jnp.int32)[None, :] + offset).astype(jnp.int32)
    pre_norm_gain = 1.0 + 0.02 * jax.random.normal(ks[3], (DEPTH, D_MODEL), jnp.float32)
    post_norm_gain = 1.0 + 0.02 * jax.random.normal(ks[4], (DEPTH, D_MODEL), jnp.float32)
    w_ada = jax.random.normal(ks[5], (DEPTH, D_MODEL, 3 * D_MODEL), jnp.float32) * D_MODEL ** -0.5
    b_ada = 0.01 * jax.random.normal(ks[6], (DEPTH, 3 * D_MODEL), jnp.float32)
    w_in = jax.random.normal(ks[7], (DEPTH, D_MODEL, D_IN), jnp.float32) * D_MODEL ** -0.5
    w_out = jax.random.normal(ks[8], (DEPTH, D_MIX, D_MODEL), jnp.float32) * D_MIX ** -0.5
    return {"x": x, "c": c, "positions": positions,
            "pre_norm_gain": pre_norm_gain, "post_norm_gain": post_norm_gain,
            "w_ada": w_ada, "b_ada": b_ada, "w_in": w_in, "w_out": w_out}


def reference(x, c, positions, pre_norm_gain, post_norm_gain, w_ada, b_ada, w_in, w_out):
    cos, sin = rope_tables(positions)
    for layer in range(DEPTH):
        x = hybrid_layer(x, c, cos, sin, pre_norm_gain[layer], post_norm_gain[layer],
                         w_ada[layer], b_ada[layer], w_in[layer], w_out[layer])
    return x
```

```python
import contextlib
import os
_SUB = int(os.environ.get('SUB', '99'))
_RSUB = int(os.environ.get('RSUB', '99'))
_ASUB = int(os.environ.get('ASUB', '99'))
_BSUB = int(os.environ.get('BSUB', '99'))
_GSUB = int(os.environ.get('GSUB', '99'))
import math
import numpy as np
import concourse.bass as bass
import concourse.mybir as mybir
from concourse.bass_utils import run_bass_kernel_spmd

F32 = mybir.dt.float32
BF = mybir.dt.bfloat16
I32 = mybir.dt.int32
AF = mybir.ActivationFunctionType
ALU = mybir.AluOpType
AX = mybir.AxisListType

NCORES = 8
S = 2048
D = 1024
NT = 16
BIG = 30000.0
EPS = 1e-6
ENGS = ("sync", "scalar", "vector", "gpsimd", "tensor")


class Op:
    __slots__ = ("eng", "fn", "deps", "is_dma", "semkey", "signal", "sigval")

    def __init__(self, eng, fn, is_dma, semkey):
        self.eng = eng
        self.fn = fn
        self.deps = []
        self.is_dma = is_dma
        self.semkey = semkey
        self.signal = False
        self.sigval = None


class Prog:
    def __init__(self):
        self.q = {e: [] for e in ENGS}
        self.lastw = {}
        self.readers = {}
        self.fence_deps = {}
        self.last_dma = {}

    def op(self, eng, fn, reads=(), writes=(), dma=False, semkey=None, extra=()):
        o = Op(eng, fn, dma, semkey)
        deps = {}
        for k in reads:
            w = self.lastw.get(k)
            if w is not None:
                deps[id(w)] = w
        for k in writes:
            w = self.lastw.get(k)
            if w is not None:
                deps[id(w)] = w
            for r in self.readers.get(k, {}).values():
                deps[id(r)] = r
        for d in extra:
            if d is not None:
                deps[id(d)] = d
        fd = self.fence_deps.pop(eng, None)
        if fd:
            for d in fd:
                deps[id(d)] = d
        o.deps = list(deps.values())
        rk = ("dma", semkey) if dma else eng
        for k in reads:
            self.readers.setdefault(k, {})[rk] = o
        for k in writes:
            self.lastw[k] = o
            self.readers[k] = {}
        self.q[eng].append(o)
        if dma:
            self.last_dma[semkey] = o
        return o

    def fence(self):
        last = [self.q[e][-1] for e in ENGS if self.q[e] and not self.q[e][-1].is_dma]
        last += list(self.last_dma.values())
        for e in ENGS:
            self.fence_deps[e] = list(last)

    def finalize(self):
        for e in ENGS:
            for o in self.q[e]:
                for d in o.deps:
                    if d.eng == "tensor" and e == "tensor" and not d.is_dma and not o.is_dma:
                        continue
                    d.signal = True
        self.semkeys = {}
        for e in ENGS:
            cnt = {}
            for o in self.q[e]:
                if not o.signal:
                    continue
                key = ("dma", o.semkey) if o.is_dma else ("eng", e)
                cnt[key] = cnt.get(key, 0) + (16 if o.is_dma else 1)
                o.sigval = (key, cnt[key])
                self.semkeys[key] = None

    def emit(self, block, sems):
        for e in ENGS:
            ops = self.q[e]
            if not ops:
                continue

            def body(eng, ops=ops, e=e):
                waited = {}
                for o in ops:
                    need = {}
                    for d in o.deps:
                        if d.eng == "tensor" and e == "tensor" and not d.is_dma and not o.is_dma:
                            continue
                        k, v = d.sigval
                        if need.get(k, 0) < v:
                            need[k] = v
                    for k, v in need.items():
                        if waited.get(k, 0) < v:
                            eng.wait_ge(sems[k], v)
                            waited[k] = v
                    if o.fn is None:
                        continue
                    ins = o.fn(eng)
                    if o.signal:
                        ins.then_inc(sems[o.sigval[0]], 16 if o.is_dma else 1)

            getattr(block, e)(body)


def _prod(xs):
    r = 1
    for v in xs:
        r *= v
    return r


def ap(t, off, dims, np_=128, p0=0):
    ps = _prod(list(t.shape)[1:])
    return bass.AP(t, p0 * ps + off, [[ps, np_]] + [list(d) for d in dims])


class _Stop(Exception):
    pass


def build_program(nseq=2, depth=2, dbg=None, stop=None):
    nc = bass.Bass("TRN2", target_bir_lowering=False)
    dt_in = lambda n, s, d=F32: nc.dram_tensor(n, s, d, kind="ExternalInput").ap()
    x_d = dt_in("x", [nseq, S, D])
    cT_d = dt_in("cT", [128, 8, 2])
    pos_d = dt_in("pos", [128, 2, NT], I32)
    pregT_d = dt_in("pregT", [128, 2, 8])
    badaT_d = dt_in("badaT", [128, 2, 16])
    badag_d = dt_in("badag", [2, D])
    postg_d = dt_in("postg", [2, D])
    wada_d = dt_in("w_ada", [2, D, 3 * D])
    win_d = dt_in("w_in", [2, D, 4 * D])
    wout_d = dt_in("w_out", [2, D, D])
    masks_d = dt_in("masks", [128, 13, 128])
    invf_d = dt_in("invf", [128, 8])
    gbias_d = dt_in("gbias", [128, 16, 8])
    cb_d = dt_in("cbias", [128, 16, 8])
    onehot_d = dt_in("onehot", [8, 4, S])
    out_d = nc.dram_tensor("out", [nseq, S, D], F32, kind="ExternalOutput").ap()
    gp_d = nc.dram_tensor("gp_scr", [2, 2, 128, D], F32).ap()
    dbg_d = {}
    if dbg:
        for name, shape in dbg.items():
            dbg_d[name] = nc.dram_tensor("dbg_" + name, shape, F32 if name in ("accA", "accN") else BF, kind="ExternalOutput").ap()

    P = Prog()
    es = contextlib.ExitStack()
    with es:
        sb = lambda n, s, d: es.enter_context(nc.sbuf_tensor("sb_" + n, s, d))
        pp = lambda n, s, d: es.enter_context(nc.psum_tensor("ps_" + n, s, d))
        hT = sb("hT", [128, 8, S], BF)
        OT = sb("OT", [128, 8, S], BF)
        Wb = sb("Wb", [128, 8, 1024], BF)
        QK = sb("QK", [128, 8, S], BF)
        Vt = sb("Vt", [128, NT, 4, 128], BF)
        accA = sb("accA", [128, S], F32)
        ptb = sb("ptb", [128, 4, 640], BF)
        stag = sb("stag", [128, 2, 576], BF)
        augst = sb("augst", [128, 4, 4, 72], BF)
        xt = sb("xt", [128, 4, D], F32)
        xn = sb("xn", [128, 4, D], BF)
        gpb = sb("gpb", [128, D], F32)
        rdt = sb("rdt", [128, 2, 512], F32)
        fT = sb("fT", [128, 2, 512], F32)
        masks = sb("masks", [128, 13, 128], BF)
        ident = sb("ident", [128, 128], BF)
        identf = sb("identf", [128, 128], F32)
        cs2 = sb("cs2", [128, NT, 16], F32)
        sn2 = sb("sn2", [128, NT, 16], F32)
        invf = sb("invf", [128, 8], F32)
        gbias = sb("gbias", [128, 16, 8], F32)
        cbias = sb("cbias", [128, 16, 8], F32)
        cTs = sb("cTs", [128, 8, 2], F32)
        scs = sb("scs", [128, 8, 2], BF)
        pregT = sb("pregT", [128, 2, 8], F32)
        badaT = sb("badaT", [128, 2, 16], F32)
        ABt = sb("ABt", [128, 2, 2, 2, 8], F32)
        tmp8 = sb("tmp8", [128, 8], F32)
        posi = sb("posi", [128, 2, NT], I32)
        rp = [sb("rp%d" % i, [128, NT, 8], F32) for i in range(5)]
        rpi = sb("rpi", [128, NT, 8], I32)
        ropet = sb("ropet", [128, 2, 2, 8, 16], F32)
        rstage = sb("rstage", [128, 2, 8, 16], F32)
        ss4 = sb("ss4", [128, 8], F32)
        rstd4 = sb("rstd4", [128, 8], F32)
        ssy = sb("ssy", [128, 4], F32)
        rsy = sb("rsy", [128, 4], F32)
        junk = sb("junk", [128, D], BF)
        kmf = sb("kmf", [128, 4, 8], F32)
        kmb = sb("kmb", [128, 4, 8], BF)
        g1 = sb("g1", [128, 4, 4, 8], F32)
        cmpt = sb("cmpt", [128, 16, 8, 8], BF)
        rank = sb("rank", [128, 4, 4, 8], F32)
        selt = sb("selt", [128, 4, 4, 8], F32)
        pA = pp("pA", [128, 1024], F32)
        pB = pp("pB", [128, 1024], F32)
        pC = pp("pC", [128, 512], F32)
        pD = pp("pD", [128, 512], F32)
        pT = [pp("pT0", [128, 1024], BF), pp("pT1", [128, 1024], BF)]

        ld = lambda eng, dst, src, key, sk: P.op(eng, lambda e: e.dma_start(out=dst, in_=src),
                                                 writes=[key], dma=True, semkey=sk)
        ld("gpsimd", masks[:], masks_d, "masks", "c0")
        ld("sync", invf[:], invf_d, "invf", "c1")
        ld("sync", gbias[:], gbias_d, "gbias", "c2")
        ld("sync", cbias[:], cb_d, "cbias", "c3")
        ld("sync", cTs[:], cT_d, "cTs", "c4")
        ld("sync", pregT[:], pregT_d, "pregT", "c5")
        ld("sync", badaT[:], badaT_d, "badaT", "c6")
        ld("sync", posi[:], pos_d, "posi", "c7")
        P.op("gpsimd", lambda e: e.memset(identf[:], 0.0), writes=["identf"])
        P.op("gpsimd", lambda e: e.affine_select(out=identf[:], in_=identf[:], pattern=[[1, 128]],
                                                  compare_op=ALU.not_equal, fill=1.0, base=0,
                                                  channel_multiplier=-1),
             reads=["identf"], writes=["identf"])
        P.op("vector", lambda e: e.tensor_copy(out=ident[:], in_=identf[:]), reads=["identf"], writes=["ident"])
        P.op("gpsimd", lambda e: e.memset(Vt[:, :, :, 64:128], 1.0), writes=["Vones"])
        P.op("gpsimd", lambda e: e.memset(augst[:], 0.0), writes=["augst"])

        P.op("scalar", lambda e: e.activation(out=scs[:], in_=cTs[:], func=AF.Silu), reads=["cTs"], writes=["scs"])
        screp_t = xn
        for b in range(nseq):
            P.op("vector", lambda e, b=b: e.tensor_copy(
                out=ap(screp_t, b * 1024, [[128, 8], [1, 128]]),
                in_=ap(scs, b, [[2, 8], [0, 128]])), reads=["scs"], writes=[("screp", b)])
        for l in range(depth):
            ld("sync", accA[:, 0:D], badag_d[l, :].partition_broadcast(128), "accA", "c8")
            ld("sync", accA[:, D:2 * D], postg_d[l, :].partition_broadcast(128), "accA", "c8")
            for g in range(6):
                src = wada_d[l].rearrange("(kc p) c -> p kc c", p=128)[:, :, g * 512:(g + 1) * 512]
                wi = (l * 6 + g) % 2
                Wst = Wb[:, :, wi * 512:(wi + 1) * 512]
                wk = "Wst%d" % wi
                P.op("gpsimd", lambda e, src=src, Wst=Wst: e.dma_start(out=Wst, in_=src), writes=[wk], dma=True,
                     semkey="wst%d" % wi)
                if g < 4:
                    for fc in range(4):
                        Fi = g * 4 + fc
                        for kc in range(8):
                            P.op("tensor", lambda e, Fi=Fi, fc=fc, kc=kc, Wst=Wst: e.matmul(
                                pC[:, Fi * 2:Fi * 2 + nseq], lhsT=Wst[:, kc, fc * 128:(fc + 1) * 128],
                                rhs=scs[:, kc, 0:nseq], start=(kc == 0), stop=(kc == 7)),
                                reads=[wk, "scs"], writes=["pC"])
                else:
                    for b in range(nseq):
                        bank = pA if b == 0 else pB
                        bk = "pA0" if b == 0 else "pB0"
                        for kc in range(8):
                            P.op("tensor", lambda e, b=b, kc=kc, bank=bank, Wst=Wst: e.matmul(
                                bank[:, 0:512],
                                lhsT=ap(screp_t, b * 1024 + kc * 128, [[1, 128]]),
                                rhs=Wst[:, kc, :], start=(kc == 0), stop=(kc == 7)),
                                reads=[wk, ("screp", b)], writes=[bk])
                        c0 = (g - 4) * 512
                        gt = gpb[:, b * 512:(b + 1) * 512]
                        P.op("vector", lambda e, bank=bank, c0=c0, gt=gt: e.tensor_tensor(
                            out=gt, in0=bank[:, 0:512], in1=accA[:, c0:c0 + 512], op=ALU.add),
                            reads=[bk, "accA"], writes=[("gpb", b)])
                        P.op("vector", lambda e, c0=c0, gt=gt: e.tensor_tensor(
                            out=gt, in0=gt, in1=accA[:, D + c0:D + c0 + 512], op=ALU.mult),
                            reads=[("gpb", b), "accA"], writes=[("gpb", b)])
                        P.op("sync", lambda e, l=l, b=b, c0=c0, gt=gt: e.dma_start(
                            out=gp_d[l, b, :, c0:c0 + 512], in_=gt),
                            reads=[("gpb", b)], writes=[("gpd", l, b)], dma=True, semkey="gpst%d" % b)
            for b in range(nseq):
                P.op("vector", lambda e, l=l, b=b: e.tensor_tensor(
                    out=ABt[:, l, b, 1, :], in0=ap(pC, b, [[2, 8]]), in1=badaT[:, l, 0:8], op=ALU.add),
                    reads=["pC", "badaT"], writes=[("ABt", l, b)])
                P.op("vector", lambda e, l=l, b=b: e.tensor_tensor(
                    out=tmp8[:], in0=ap(pC, 16 + b, [[2, 8]]), in1=badaT[:, l, 8:16], op=ALU.add),
                    reads=["pC", "badaT"], writes=["tmp8"])
                P.op("vector", lambda e, l=l, b=b: e.scalar_tensor_tensor(
                    out=ABt[:, l, b, 0, :], in0=tmp8[:], scalar=1.0, in1=pregT[:, l, :],
                    op0=ALU.add, op1=ALU.mult),
                    reads=["tmp8", "pregT"], writes=[("ABt", l, b)])
        P.fence()

        TWO_PI = 2.0 * math.pi
        C1 = 6.28125
        C2 = TWO_PI - C1

        def rope_tables(b):
            posf, ang, a2, kf, r = rp
            vop = lambda fn, reads, writes: P.op("vector", fn, reads=reads, writes=writes)
            vop(lambda e: e.tensor_copy(out=ang[:, :, 0], in_=posi[:, b, :]), ["posi"], ["r_posf"])
            vop(lambda e: e.tensor_tensor(out=posf[:], in0=ap(ang, 0, [[8, NT], [0, 8]]),
                                          in1=ap(invf, 0, [[0, NT], [1, 8]]), op=ALU.mult),
                ["r_posf", "invf"], ["r_ang"])
            for which, shift in ((0, 0.0), (1, 0.5 * math.pi)):
                vop(lambda e, shift=shift: e.tensor_scalar(out=a2[:], in0=posf[:], scalar1=shift, scalar2=None,
                                                           op0=ALU.add), ["r_ang"], ["r_a2"])
                vop(lambda e: e.tensor_scalar(out=rpi[:], in0=a2[:], scalar1=1.0 / TWO_PI, scalar2=None,
                                              op0=ALU.mult), ["r_a2"], ["r_ki"])
                vop(lambda e: e.tensor_copy(out=kf[:], in_=rpi[:]), ["r_ki"], ["r_kf"])
                vop(lambda e: e.scalar_tensor_tensor(out=r[:], in0=kf[:], scalar=-C1, in1=a2[:],
                                                     op0=ALU.mult, op1=ALU.add), ["r_kf", "r_a2"], ["r_r"])
                vop(lambda e: e.scalar_tensor_tensor(out=r[:], in0=kf[:], scalar=-C2, in1=r[:],
                                                     op0=ALU.mult, op1=ALU.add), ["r_kf", "r_r"], ["r_r"])
                vop(lambda e: e.tensor_scalar(out=kf[:], in0=r[:], scalar1=math.pi, scalar2=-TWO_PI,
                                              op0=ALU.is_gt, op1=ALU.mult), ["r_r"], ["r_kf"])
                vop(lambda e: e.tensor_tensor(out=r[:], in0=r[:], in1=kf[:], op=ALU.add), ["r_kf", "r_r"], ["r_r"])
                vop(lambda e: e.tensor_scalar(out=kf[:], in0=r[:], scalar1=-math.pi, scalar2=TWO_PI,
                                              op0=ALU.is_lt, op1=ALU.mult), ["r_r"], ["r_kf"])
                vop(lambda e: e.tensor_tensor(out=r[:], in0=r[:], in1=kf[:], op=ALU.add), ["r_kf", "r_r"], ["r_r"])
                vop(lambda e: e.tensor_scalar(out=r[:], in0=r[:], scalar1=3.1415925, scalar2=-3.1415925,
                                              op0=ALU.min, op1=ALU.max), ["r_r"], ["r_r"])
                if which == 0:
                    P.op("scalar", lambda e: e.activation(out=sn2[:, :, 8:16], in_=r[:], func=AF.Sin),
                         reads=["r_r"], writes=["sn2"])
                    P.op("scalar", lambda e: e.mul(out=sn2[:, :, 0:8], in_=sn2[:, :, 8:16], mul=-1.0),
                         reads=["sn2"], writes=["sn2"])
                else:
                    P.op("scalar", lambda e: e.activation(out=cs2[:, :, 0:8], in_=r[:], func=AF.Sin),
                         reads=["r_r"], writes=["cs2"])
                    P.op("scalar", lambda e: e.copy(out=cs2[:, :, 8:16], in_=cs2[:, :, 0:8]),
                         reads=["cs2"], writes=["cs2"])

        banks = {"pA0": pA[:, 0:512], "pA1": pA[:, 512:1024], "pB0": pB[:, 0:512], "pB1": pB[:, 512:1024],
                 "pC": pC[:], "pD": pD[:]}

        def dump(name, src_ap, key):
            if name in dbg_d:
                P.op("sync", lambda e: e.dma_start(out=dbg_d[name], in_=src_ap), reads=key, dma=True,
                     semkey="dbg_" + name, writes=[("dbgout", name)])

        def load_wslice(l, mixer, hf):
            base = mixer * 2048
            wv = win_d[l].rearrange("(kc p) c -> p kc c", p=128)
            for i in range(4):
                c0 = base + i * 512 + hf * 256
                P.op("gpsimd", lambda e, i=i, c0=c0: e.dma_start(out=Wb[:, :, i * 256:(i + 1) * 256],
                                                                   in_=wv[:, :, c0:c0 + 256]),
                     writes=["Wb"], dma=True, semkey="wb")

        def norm_phase(b, l, xsrc):
            for hg in range(8):
                par = hg % 2
                for t2 in range(2):
                    sl = par * 2 + t2
                    tt_ = hg * 2 + t2
                    P.op("sync", lambda e, sl=sl, tt_=tt_: e.dma_start(out=xt[:, sl, :], in_=xsrc[b, tt_ * 128:(tt_ + 1) * 128, :]),
                         reads=[("xd", b, tt_)], writes=[("xt", sl)], dma=True, semkey="xt%d" % sl)
                for t2 in range(2):
                    sl = par * 2 + t2
                    P.op("scalar", lambda e, sl=sl: e.activation(out=junk[:], in_=xt[:, sl, :], func=AF.Square,
                                                                  accum_out=ss4[:, sl:sl + 1]),
                         reads=[("xt", sl)], writes=["junk", ("ss4", par)])
                P.op("scalar", lambda e, par=par: e.activation(out=ss4[:, 4 + 2 * par:6 + 2 * par], in_=ss4[:, 2 * par:2 * par + 2],
                                                                func=AF.Sqrt, bias=EPS_AP, scale=1.0 / D),
                     reads=[("ss4", par)], writes=[("ss4b", par)])
                P.op("vector", lambda e, par=par: e.reciprocal(out=rstd4[:, 2 * par:2 * par + 2], in_=ss4[:, 4 + 2 * par:6 + 2 * par]),
                     reads=[("ss4b", par)], writes=[("rstd4", par)])
                for t2 in range(2):
                    sl = par * 2 + t2
                    P.op("vector", lambda e, sl=sl: e.tensor_scalar(out=xn[:, sl, :], in0=xt[:, sl, :],
                                                                     scalar1=rstd4[:, sl:sl + 1], scalar2=None, op0=ALU.mult),
                         reads=[("xt", sl), ("rstd4", par)], writes=[("xn", sl)])
                for c in range(8):
                    pt_ = pT[c % 2]
                    pk = ("pT", c % 2)
                    for t2 in range(2):
                        sl = par * 2 + t2
                        P.op("tensor", lambda e, c=c, t2=t2, sl=sl, pt_=pt_: e.transpose(
                            pt_[:, t2 * 128:(t2 + 1) * 128], xn[:, sl, c * 128:(c + 1) * 128], ident[:]),
                            reads=[("xn", sl), "ident"], writes=[pk])
                    P.op("vector", lambda e, c=c, pt_=pt_, hg=hg: e.tensor_scalar(
                        out=hT[:, c, hg * 256:(hg + 1) * 256], in0=pt_[:, 0:256],
                        scalar1=ABt[:, l, b, 0, c:c + 1], scalar2=ABt[:, l, b, 1, c:c + 1],
                        op0=ALU.mult, op1=ALU.add),
                        reads=[pk, ("ABt", l, b)], writes=[("hT", hg // 2)])

        def proj_tile_mm(tt, mixer):
            qb, qk_ = (pA[:, 0:512], "pA0") if tt % 2 == 0 else (pA[:, 512:1024], "pA1")
            vb, vk = (pB[:, 0:512], "pB0") if tt % 2 == 0 else (pB[:, 512:1024], "pB1")
            for kc in range(8):
                P.op("tensor", lambda e, kc=kc: e.matmul(qb, lhsT=hT[:, kc, tt * 128:(tt + 1) * 128],
                                                          rhs=Wb[:, kc, 0:512], start=(kc == 0), stop=(kc == 7)),
                     reads=[("hT", tt // 4), "Wb"], writes=[qk_])
            for kc in range(8):
                P.op("tensor", lambda e, kc=kc: e.matmul(vb[:, 0:256], lhsT=hT[:, kc, tt * 128:(tt + 1) * 128],
                                                          rhs=Wb[:, kc, 512:768], start=(kc == 0), stop=(kc == 7)),
                     reads=[("hT", tt // 4), "Wb"], writes=[vk])
            return qb, qk_, vb, vk

        def proj_tile_post(tt, mixer, qb, qk_, vb, vk):
            W = 64 if mixer == 0 else 72
            if _SUB < 1:
                return
            sbuf_i = tt % 2
            sk = ("stag", sbuf_i)
            st = lambda off, dims: ap(stag, sbuf_i * 576 + off, dims)
            psv = lambda off, dims: bass.AP(qb.tensor, qb.offset + off, [list(qb.ap[0])] + [list(d) for d in dims])
            P.op("scalar", lambda e: e.activation(out=st(0, [[W, 8], [1, 64]]), in_=psv(0, [[64, 8], [1, 64]]),
                                                  func=AF.Copy), reads=[qk_], writes=[sk])
            if _SUB < 2:
                return
            ra = lambda off, dims: ap(ropet, sbuf_i * 256 + off, dims)
            rk = ("ropet", sbuf_i)
            rsk = ("rstage", sbuf_i)
            rs_ = lambda off, dims: ap(rstage, sbuf_i * 128 + off, dims)
            P.op("scalar", lambda e: e.activation(out=rs_(0, [[16, 8], [1, 16]]), in_=psv(0, [[64, 8], [1, 16]]),
                                                  func=AF.Copy), reads=[qk_], writes=[rsk])
            P.op("vector", lambda e: e.tensor_tensor(out=ra(0, [[16, 8], [1, 16]]), in0=rs_(0, [[16, 8], [1, 16]]),
                                                     in1=ap(cs2, tt * 16, [[0, 8], [1, 16]]), op=ALU.mult),
                 reads=[rsk, "cs2"], writes=[rk])
            if _RSUB < 2:
                return
            P.op("vector", lambda e: e.tensor_tensor(out=ra(128, [[16, 8], [1, 8]]), in0=rs_(8, [[16, 8], [1, 8]]),
                                                     in1=ap(sn2, tt * 16, [[0, 8], [1, 8]]), op=ALU.mult),
                 reads=[rsk, "sn2"], writes=[rk])
            P.op("vector", lambda e: e.tensor_tensor(out=ra(128 + 8, [[16, 8], [1, 8]]), in0=rs_(0, [[16, 8], [1, 8]]),
                                                     in1=ap(sn2, tt * 16 + 8, [[0, 8], [1, 8]]), op=ALU.mult),
                 reads=[rsk, "sn2"], writes=[rk])
            if _RSUB < 3:
                return
            P.op("vector", lambda e: e.tensor_tensor(out=st(0, [[W, 8], [1, 16]]), in0=ra(0, [[16, 8], [1, 16]]),
                                                     in1=ra(128, [[16, 8], [1, 16]]), op=ALU.add),
                 reads=[rk, sk], writes=[sk])
            if _SUB < 3:
                return
            vps = bass.AP(vb.tensor, vb.offset, [list(vb.ap[0]), [64, 4], [1, 64]])
            P.op("scalar", lambda e: e.activation(out=Vt[:, tt, :, 0:64], in_=vps, func=AF.Copy),
                 reads=[vk], writes=[("V", tt)])
            if _SUB < 4:
                return
            pt_ = pT[tt % 2]
            pk = ("pT", tt % 2)
            if mixer == 0:
                for i in range(4):
                    P.op("tensor", lambda e, i=i: e.transpose(pt_[:, i * 128:(i + 1) * 128],
                                                              st(i * 128, [[1, 128]]), ident[:]),
                         reads=[sk, "ident"], writes=[pk])
                P.op("vector", lambda e: e.tensor_copy(out=ap(QK, tt * 128, [[S, 4], [1, 128]]),
                                                       in_=ap(pt_, 0, [[128, 4], [1, 128]])),
                     reads=[pk], writes=[("QK", s_, tt) for s_ in range(4)])
            else:
                for i in range(8):
                    P.op("tensor", lambda e, i=i: e.transpose(pt_[0:64, i * 128:(i + 1) * 128],
                                                              st(i * 72, [[1, 64]]), ident[:]),
                         reads=[sk, "ident"], writes=[pk])
                P.op("vector", lambda e: e.tensor_copy(out=ap(QK, tt * 128, [[S, 8], [1, 128]], np_=64),
                                                       in_=ap(pt_, 0, [[128, 8], [1, 128]], np_=64)),
                     reads=[pk], writes=[("QK", s_, tt) for s_ in range(8)] + ["QKhi_A"])
                if tt % 2 == 1:
                    n_ = tt // 2
                    P.op("vector", lambda e: e.tensor_reduce(
                        out=ap(kmf, n_, [[8, 4]], np_=64), in_=ap(QK, 4 * S + n_ * 256, [[S, 4], [1, 256]], np_=64),
                        axis=AX.X, op=ALU.add),
                        reads=[("QK", s_, t_) for s_ in range(4, 8) for t_ in (tt - 1, tt)], writes=["kmf"])

        def proj_phase(b, l, mixer, hf):
            prev = None
            for tt in range(NT):
                cur = proj_tile_mm(tt, mixer)
                if prev is not None:
                    proj_tile_post(tt - 1, mixer, *prev)
                prev = cur
            proj_tile_post(NT - 1, mixer, *prev)
            if _SUB < 5:
                return
            for pr in range(2):
                ch = mixer * 4 + hf * 2 + pr
                for tc in range(4):
                    bank, bk = (pC[:], "pC") if (pr * 4 + tc) % 2 == 0 else (pD[:], "pD")
                    for kc in range(8):
                        P.op("tensor", lambda e, kc=kc, pr=pr, tc=tc, bank=bank: e.matmul(
                            bank, lhsT=Wb[:, kc, 768 + pr * 128:768 + (pr + 1) * 128],
                            rhs=hT[:, kc, tc * 512:(tc + 1) * 512], start=(kc == 0), stop=(kc == 7)),
                            reads=[("hT", tc), "Wb"], writes=[bk])
                    P.op("scalar", lambda e, ch=ch, tc=tc, bank=bank: e.activation(
                        out=OT[:, ch, tc * 512:(tc + 1) * 512], in_=bank, func=AF.Silu),
                        reads=[bk], writes=[("OT", ch, tc)])
            if _SUB < 6:
                return
            if mixer == 0:
                P.op("gpsimd", lambda e: e.memset(ap(QK, 4 * S + 64, [[128, 64], [1, 64]]), 1.0),
                     writes=[("QK", s_, t_) for s_ in range(4, 8) for t_ in range(NT)] + ["QKhi_A"])
                for r in range(16):
                    bank, bk = (pC[:], "pC") if r % 2 == 0 else (pD[:], "pD")
                    for kc in range(8):
                        P.op("tensor", lambda e, kc=kc, r=r, bank=bank: e.matmul(
                            bank[:, 0:256], lhsT=ap(hT, kc * S + r, [[16, 128]]), rhs=Wb[:, kc, 512:768],
                            start=(kc == 0), stop=(kc == 7)),
                            reads=[("hT", 0), ("hT", 1), ("hT", 2), ("hT", 3), "Wb"], writes=[bk])
                    P.op("vector", lambda e, r=r, bank=bank: e.tensor_copy(
                        out=ap(QK, 4 * S + r * 512, [[128, 4], [1, 64]]),
                        in_=bass.AP(bank.tensor, bank.offset, [list(bank.ap[0]), [64, 4], [1, 64]])),
                        reads=[bk], writes=[("Vp", r)], extra=[P.lastw.get(("QK", 4, 0))])

        def normalize(j, src_bank, src_key, ncols, ch, col0, which):
            pb = (j % 2) * 64
            db = 64 - pb
            rd = ap(rdt, which * 512, [[1, ncols]], np_=64, p0=pb)
            ff = ap(fT, which * 512, [[1, ncols]], np_=64, p0=pb)
            srcn = bass.AP(src_bank.tensor, src_bank.offset + pb * src_bank.ap[0][0], [[src_bank.ap[0][0], 64], [1, ncols]])
            srcd = bass.AP(src_bank.tensor, src_bank.offset + db * src_bank.ap[0][0], [[src_bank.ap[0][0], 64], [1, ncols]])
            P.op("vector", lambda e: e.reciprocal(out=rd, in_=srcd), reads=[src_key], writes=[("rdt", which)])
            P.op("vector", lambda e: e.tensor_tensor(out=ff, in0=srcn, in1=rd, op=ALU.mult),
                 reads=[src_key, ("rdt", which)], writes=[("fT", which)])
            ot = ap(OT, ch * S + col0, [[1, ncols]], np_=64, p0=pb)
            P.op("gpsimd", lambda e: e.tensor_tensor(out=ot, in0=ot, in1=ff, op=ALU.mult),
                 reads=[("fT", which)] + [("OT", ch, tc) for tc in range(col0 // 512, (col0 + ncols - 1) // 512 + 1)],
                 writes=[("OT", ch, tc) for tc in range(col0 // 512, (col0 + ncols - 1) // 512 + 1)])

        def vaug(src, tile_off, j):
            off = tile_off + j * 128 - (64 if j % 2 else 0)
            return ap(src, off, [[1, 128]])

        def attn_A(b, l, hf):
            ptn = [0]

            def head(j):
                pr = j // 2
                pb = (j % 2) * 64
                ch = hf * 2 + pr
                qT = lambda c0, n, step=1: ap(QK, pr * S + c0, [[step, n]], np_=64, p0=pb)
                kT = lambda c0, n, step=1: ap(QK, (2 + pr) * S + c0, [[step, n]], np_=64, p0=pb)
                qkeys = lambda ts: [("QK", pr, t) for t in ts]
                kkeys = lambda ts: [("QK", 2 + pr, t) for t in ts]
                units = []
                for i in range(NT):
                    kts = list(range(max(0, i - 4), i + 1))
                    units.append((i, kts))

                def score(u, ui):
                    i, kts = u
                    sb_, keys = (pA, ["pA0", "pA1"]) if ui % 2 == 0 else (pB, ["pB0", "pB1"])
                    n = len(kts)
                    n0 = min(n, 4)
                    P.op("tensor", lambda e: e.matmul(sb_[:, 0:n0 * 128], lhsT=ident[:],
                                                      rhs=ap(masks, (5 - n) * 128, [[1, n0 * 128]]), start=True, stop=False),
                         reads=["ident", "masks"], writes=[keys[0]])
                    if n == 5:
                        P.op("tensor", lambda e: e.matmul(sb_[:, 512:640], lhsT=ident[:],
                                                          rhs=ap(masks, 4 * 128, [[1, 128]]), start=True, stop=False),
                             reads=["ident", "masks"], writes=[keys[1]])
                    for s_, kt in enumerate(kts):
                        P.op("tensor", lambda e, s_=s_, kt=kt: e.matmul(
                            sb_[:, s_ * 128:(s_ + 1) * 128], lhsT=kT(kt * 128, 128), rhs=qT(i * 128, 128),
                            start=False, stop=(s_ == n0 - 1 or s_ == 4)),
                            reads=qkeys([i]) + kkeys([kt]), writes=[keys[s_ // 4]])
                    return sb_, keys

                def softmax_pv(u, ui, sb_, keys):
                    i, kts = u
                    n = len(kts)
                    pi = ptn[0] % 3
                    ptn[0] += 1
                    pt_ = ap(ptb, pi * 640, [[1, n * 128]])
                    P.op("scalar", lambda e: e.activation(out=pt_, in_=sb_[:, 0:n * 128], func=AF.Exp, scale=0.125),
                         reads=keys if n > 4 else keys[:1], writes=[("pt", pi)])
                    if dbg and j == 0 and hf == 0 and b == 0 and l == 0 and i in (0, 5):
                        dump("pt%d" % i, ptb[:, pi, :], [("pt", pi)])
                    accb, ak = (pC, "pC") if (i // 4) % 2 == 0 else (pD, "pD")
                    for s_, kt in enumerate(kts):
                        P.op("tensor", lambda e, s_=s_, kt=kt: e.matmul(
                            accb[:, (i % 4) * 128:(i % 4 + 1) * 128], lhsT=vaug(Vt, kt * 512, j),
                            rhs=ap(ptb, pi * 640 + s_ * 128, [[1, 128]]), start=(s_ == 0), stop=(s_ == n - 1)),
                            reads=[("V", kt), "Vones", ("pt", pi)], writes=[ak])
                    if i % 4 == 3:
                        c0 = (i // 4) * 512
                        P.op("scalar", lambda e: e.activation(out=accA[:, c0:c0 + 512], in_=accb[:], func=AF.Copy),
                             reads=[ak], writes=[("accA", i // 4)])
                        if dbg and j == 0 and hf == 0 and b == 0 and l == 0 and i == 15:
                            dump("accN", accA[:], [("accA", q_) for q_ in range(4)])

                pend = score(units[0], 0)
                for ui in range(len(units)):
                    nxt = score(units[ui + 1], ui + 1) if ui + 1 < len(units) else None
                    softmax_pv(units[ui], ui, *pend)
                    pend = nxt
                if _ASUB < 1 and hf == 1:
                    return
                def score3(g):
                    sb_, key = (pA[:, 0:512], "pA0") if g % 2 == 0 else (pB[:, 0:512], "pB0")
                    P.op("tensor", lambda e: e.matmul(sb_, lhsT=ident[:], rhs=ap(masks, 9 * 128, [[1, 512]]),
                                                      start=True, stop=False),
                         reads=["ident", "masks"], writes=[key])
                    for s_ in range(4):
                        r = 4 * g + s_
                        P.op("tensor", lambda e, s_=s_, r=r: e.matmul(
                            sb_[:, s_ * 128:(s_ + 1) * 128], lhsT=kT(r, 128, 16), rhs=qT(r, 128, 16),
                            start=False, stop=(s_ == 3)),
                            reads=qkeys(range(NT)) + kkeys(range(NT)), writes=[key])
                    return sb_, key

                def pv3(g, sb_, key):
                    pi = ptn[0] % 3
                    ptn[0] += 1
                    pt_ = ap(ptb, pi * 640, [[1, 512]])
                    P.op("scalar", lambda e: e.activation(out=pt_, in_=sb_, func=AF.Exp, scale=0.125),
                         reads=[key], writes=[("pt", pi)])
                    accb, ak = (pC, "pC") if g % 2 == 0 else (pD, "pD")
                    for s_ in range(4):
                        r = 4 * g + s_
                        P.op("tensor", lambda e, s_=s_, r=r: e.matmul(
                            accb[:, s_ * 128:(s_ + 1) * 128], lhsT=vaug(QK, 4 * S + r * 512, j),
                            rhs=ap(ptb, pi * 640 + s_ * 128, [[1, 128]]), start=True, stop=True),
                            reads=[("Vp", r), ("pt", pi), "QKhi_A"], writes=[ak])
                    P.op("vector", lambda e: e.tensor_tensor(
                        out=ap(accA, 4 * g, [[1, 4], [16, 128]]), in0=ap(accA, 4 * g, [[1, 4], [16, 128]]),
                        in1=ap(accb, 0, [[128, 4], [1, 128]]), op=ALU.add),
                        reads=[ak] + [("accA", q_) for q_ in range(4)], writes=[("accA", q_) for q_ in range(4)])

                pend = score3(0)
                for g in range(4):
                    nxt = score3(g + 1) if g + 1 < 4 else None
                    pv3(g, *pend)
                    pend = nxt
                if dbg and j == 0 and hf == 0 and b == 0 and l == 0:
                    dump("accA", accA[:], [("accA", q_) for q_ in range(4)])
                if _ASUB < 2 and hf == 1:
                    return
                for q_ in range(4):
                    normalize(j, accA[:, q_ * 512:(q_ + 1) * 512], ("accA", q_), 512, ch, q_ * 512, q_ % 2)

            for j in range(4):
                head(j)

        def attn_B(b, l, hf):
            P.op("gpsimd", lambda e: e.dma_start(out=ap(QK, 4 * S, [[S, 4], [1, S]], np_=8, p0=64), in_=onehot_d),
                 writes=[("QKaugK",)] , dma=True, semkey="onehot",
                 extra=[P.lastw.get(("QK", s_, t_)) for s_ in range(4, 8) for t_ in (0, NT - 1)])
            if _BSUB < 1:
                return
            P.op("vector", lambda e: e.tensor_scalar(out=kmb[0:64, :, :], in0=kmf[0:64, :, :], scalar1=1.0 / 256,
                                                     scalar2=None, op0=ALU.mult), reads=["kmf"], writes=["kmb"])
            if _BSUB < 2:
                return
            for tt in range(NT):
                for j in range(4):
                    P.op("tensor", lambda e, j=j, tt=tt: e.matmul(pC[:, tt * 32 + j * 8:tt * 32 + (j + 1) * 8],
                                                                  lhsT=ap(QK, j * S + tt * 128, [[1, 128]], np_=64),
                                                                  rhs=kmb[0:64, j, :], start=True, stop=True),
                         reads=[("QK", j, tt), "kmb"], writes=["pC"])
            P.op("tensor", lambda e: e.matmul(pD[:, 0:128], lhsT=ident[:], rhs=ident[:], start=True, stop=True),
                 reads=["ident"], writes=["pC", "pD"])
            gall = fT[:, 0, :]
            P.op("vector", lambda e: e.tensor_copy(out=gall, in_=pC[:]), reads=["pC"], writes=[("fT", 0)])
            def gate_group(gq):
                t0_ = 4 * gq
                g1v = ap(g1, 0, [[1, 128]])
                P.op("vector", lambda e: e.tensor_tensor(out=ap(g1, 0, [[32, 4], [8, 4], [1, 8]]),
                                                         in0=ap(fT, t0_ * 32, [[32, 4], [8, 4], [1, 8]]),
                                                         in1=ap(gbias, t0_ * 8, [[8, 4], [0, 4], [1, 8]]), op=ALU.add),
                     reads=[("fT", 0), "gbias"], writes=["g1"])
                P.op("vector", lambda e: e.tensor_tensor(
                    out=ap(cmpt, 0, [[64, 16], [8, 8], [1, 8]]),
                    in0=ap(g1, 0, [[8, 16], [0, 8], [1, 8]]), in1=ap(g1, 0, [[8, 16], [1, 8], [0, 8]]),
                    op=ALU.is_gt), reads=["g1"], writes=["cmpt"])
                P.op("vector", lambda e: e.tensor_reduce(out=ap(rank, 0, [[8, 16], [1, 8]]),
                                                         in_=ap(cmpt, 0, [[64, 16], [8, 8], [1, 8]]),
                                                         axis=AX.X, op=ALU.add),
                     reads=["cmpt"], writes=["rank"])
                P.op("vector", lambda e: e.tensor_scalar(out=ap(selt, 0, [[1, 128]]), in0=ap(rank, 0, [[1, 128]]),
                                                         scalar1=3.5, scalar2=-BIG, op0=ALU.is_gt, op1=ALU.mult),
                     reads=["rank"], writes=["selt"])
                P.op("vector", lambda e: e.tensor_tensor(out=ap(augst, 64, [[288, 4], [72, 4], [1, 8]]),
                                                         in0=ap(selt, 0, [[32, 4], [8, 4], [1, 8]]),
                                                         in1=ap(cbias, t0_ * 8, [[8, 4], [0, 4], [1, 8]]), op=ALU.add),
                     reads=["selt", "cbias"], writes=["augst"])
                for half in range(2):
                    pt_ = pT[half]
                    pk = ("pT", half)
                    for tl in range(2):
                        for j in range(4):
                            P.op("tensor", lambda e, j=j, tl=tl, pt_=pt_, half=half: e.transpose(
                                pt_[0:72, (tl * 4 + j) * 128:(tl * 4 + j + 1) * 128],
                                ap(augst, (half * 2 + tl) * 288 + j * 72, [[1, 72]]), ident[:]),
                                reads=["augst", "ident"], writes=[pk])
                    tt0 = t0_ + half * 2
                    P.op("vector", lambda e, pt_=pt_, tt0=tt0: e.tensor_copy(
                        out=ap(QK, tt0 * 128, [[128, 2], [S, 4], [1, 128]], np_=8, p0=64),
                        in_=ap(pt_, 0, [[512, 2], [128, 4], [1, 128]], np_=8, p0=64)),
                        reads=[pk], writes=[("QKaugQ", tt0), ("QKaugQ", tt0 + 1)])

            for gq in range(4):
                gate_group(gq)
            if dbg and hf == 0 and b == 0 and l == 0:
                dump("qkB", QK[:], [("QKaugQ", t_) for t_ in range(NT)] + [("QKaugK",)])
            if _BSUB < 3:
                return
            ptn = [0]
            sbanks = [(pA[:, 0:512], "pA0"), (pA[:, 512:1024], "pA1"), (pB[:, 0:512], "pB0"), (pB[:, 512:1024], "pB1")]
            def head(j):
                pb = (j % 2) * 64
                ch = 4 + hf * 2 + j // 2
                units = []
                for c in range(4):
                    for kt in range(4 * c + 4):
                        units.append((c, kt))

                def score(u, ui):
                    c, kt = u
                    col0 = max(kt, 4 * c) * 128
                    ncols = (4 * c + 4) * 128 - col0
                    sb_, key = sbanks[ui % 4]
                    qts = list(range(col0 // 128, 4 * c + 4))
                    diag = kt >= 4 * c
                    if diag:
                        P.op("tensor", lambda e: e.matmul(
                            bass.AP(sb_.tensor, sb_.offset, [list(sb_.ap[0]), [1, ncols]]),
                            lhsT=ident[:], rhs=ap(masks, 5 * 128, [[1, ncols]]), start=True, stop=False),
                            reads=["ident", "masks"], writes=[key])
                    P.op("tensor", lambda e: e.matmul(
                        bass.AP(sb_.tensor, sb_.offset, [list(sb_.ap[0]), [1, ncols]]),
                        lhsT=ap(QK, (4 + j) * S + kt * 128, [[1, 128]], np_=72),
                        rhs=ap(QK, j * S + col0, [[1, ncols]], np_=72), start=(not diag), stop=True),
                        reads=[("QK", j, t_) for t_ in qts] + [("QKaugQ", t_) for t_ in qts]
                        + [("QK", 4 + j, kt), ("QKaugK",)], writes=[key])
                    return sb_, key, col0, ncols

                def pv(u, ui, sb_, key, col0, ncols):
                    c, kt = u
                    pi = ui % 4
                    pt_ = ap(ptb, pi * 640, [[1, ncols]])
                    P.op("scalar", lambda e: e.activation(
                        out=pt_, in_=bass.AP(sb_.tensor, sb_.offset, [list(sb_.ap[0]), [1, ncols]]),
                        func=AF.Exp, scale=0.125), reads=[key], writes=[("pt", pi)])
                    accb, ak = (pC, "pC") if c % 2 == 0 else (pD, "pD")
                    oc = col0 - 4 * c * 128
                    P.op("tensor", lambda e: e.matmul(accb[:, oc:oc + ncols], lhsT=vaug(Vt, kt * 512, j), rhs=pt_,
                                                      start=(kt == 0), stop=(kt == 4 * c + 3)),
                         reads=[("V", kt), "Vones", ("pt", pi)], writes=[ak])
                    if kt == 4 * c + 3:
                        normalize(j, accb[:], ak, 512, ch, c * 512, c % 2)

                DEPTHP = 3
                pend = []
                for ui in range(min(DEPTHP, len(units))):
                    pend.append(score(units[ui], ui))
                for ui in range(len(units)):
                    cur = pend.pop(0)
                    pv(units[ui], ui, *cur)
                    if ui + DEPTHP < len(units):
                        pend.append(score(units[ui + DEPTHP], ui + DEPTHP))

            for j in range(4):
                head(j)

        def load_wout(l):
            wv = wout_d[l].rearrange("(kc p) c -> p kc c", p=128)
            for i in range(2):
                P.op("gpsimd", lambda e, i=i: e.dma_start(out=Wb[:, :, i * 512:(i + 1) * 512],
                                                            in_=wv[:, :, i * 512:(i + 1) * 512]),
                     writes=["Wb"], dma=True, semkey="wb")

        def out_phase(b, l, xsrc):
            P.op("sync", lambda e: e.dma_start(out=gpb[:], in_=gp_d[l, b]), reads=[("gpd", l, b)],
                 writes=[("gpb", 0), ("gpb", 1)], dma=True, semkey="gpld")
            def xload(t_):
                xs_ = t_ % 2
                P.op("sync", lambda e: e.dma_start(out=xt[:, xs_, :], in_=xsrc[b, t_ * 128:(t_ + 1) * 128, :]),
                     reads=[("xd", b, t_)], writes=[("xt", xs_)], dma=True, semkey="xo%d" % xs_)

            xload(0)
            for tt in range(NT):
                yb, yk = (pA, ["pA0", "pA1"]) if tt % 2 == 0 else (pB, ["pB0", "pB1"])
                xs = tt % 2
                ts = 2 + tt % 2
                if tt + 1 < NT:
                    xload(tt + 1)
                for nb in range(2):
                    for c in range(8):
                        P.op("tensor", lambda e, nb=nb, c=c, yb=yb, tt=tt: e.matmul(
                            yb[:, nb * 512:(nb + 1) * 512], lhsT=OT[:, c, tt * 128:(tt + 1) * 128],
                            rhs=Wb[:, c, nb * 512:(nb + 1) * 512], start=(c == 0), stop=(c == 7)),
                            reads=[("OT", c, tt // 4), "Wb"], writes=[yk[nb]])
                P.op("scalar", lambda e, yb=yb, xs=xs: e.activation(out=junk[:], in_=yb[:], func=AF.Square,
                                                                     accum_out=ssy[:, xs:xs + 1]),
                     reads=yk, writes=["junk", ("ssy", xs)])
                P.op("scalar", lambda e, xs=xs: e.activation(out=ssy[:, 2 + xs:3 + xs], in_=ssy[:, xs:xs + 1], func=AF.Sqrt,
                                                              bias=EPS_AP, scale=1.0 / D),
                     reads=[("ssy", xs)], writes=[("ssyb", xs)])
                P.op("vector", lambda e, xs=xs: e.reciprocal(out=rsy[:, xs:xs + 1], in_=ssy[:, 2 + xs:3 + xs]),
                     reads=[("ssyb", xs)], writes=[("rsy", xs)])
                P.op("vector", lambda e, yb=yb, xs=xs, ts=ts: e.scalar_tensor_tensor(
                    out=xt[:, ts, :], in0=yb[:], scalar=rsy[:, xs:xs + 1], in1=gpb[:], op0=ALU.mult, op1=ALU.mult),
                    reads=yk + [("rsy", xs), ("gpb", 0), ("gpb", 1)], writes=[("xt", ts)])
                P.op("gpsimd", lambda e, xs=xs, ts=ts: e.tensor_tensor(out=xt[:, ts, :], in0=xt[:, ts, :],
                                                                        in1=xt[:, xs, :], op=ALU.add),
                     reads=[("xt", ts), ("xt", xs)], writes=[("xt", ts)])
                P.op("sync", lambda e, tt=tt, ts=ts: e.dma_start(out=out_d[b, tt * 128:(tt + 1) * 128, :], in_=xt[:, ts, :]),
                     reads=[("xt", ts)], writes=[("xd", b, tt)], dma=True, semkey="xst%d" % (tt % 2))

        epst = sb("epst", [128, 1], F32)
        P.op("vector", lambda e: e.memset(epst[:], EPS), writes=["epst"])
        P.fence()
        EPS_AP = epst[:, 0:1]

        stage = [0]

        def chk():
            stage[0] += 1
            if stop is not None and stage[0] >= stop:
                raise _Stop()

        def main_loop():
          for b in range(nseq):
            chk()
            rope_tables(b)
            chk()
            for l in range(depth):
                xsrc = x_d if l == 0 else out_d
                load_wslice(l, 0, 0)
                norm_phase(b, l, xsrc)
                chk()
                if dbg and b == 0 and l == 0:
                    dump("hT", hT[:], [("hT", g_) for g_ in range(4)])
                for mixer in range(2):
                    for hf in range(2):
                        proj_phase(b, l, mixer, hf)
                        chk()
                        nm, nh = (mixer, hf + 1) if hf == 0 else (mixer + 1, 0)
                        if nm < 2:
                            load_wslice(l, nm, nh)
                        else:
                            load_wout(l)
                        if dbg and b == 0 and l == 0 and hf == 0:
                            dump("qk%d" % mixer, QK[:], [("QK", s_, t_) for s_ in range(8) for t_ in range(NT)] + [("Vp", r_) for r_ in range(16)])
                            dump("v%d" % mixer, Vt[:], [("V", t_) for t_ in range(NT)] + ["Vones"])
                        if mixer == 0:
                            attn_A(b, l, hf)
                        else:
                            attn_B(b, l, hf)
                        chk()
                if dbg and b == 0 and l == 0:
                    dump("OT", OT[:], [("OT", c_, t_) for c_ in range(8) for t_ in range(4)])
                out_phase(b, l, xsrc)
        try:
            main_loop()
        except _Stop:
            pass
        P.fence()
        P.op("sync", None)
        P.finalize()
        sems = {k: es.enter_context(nc.semaphore("s_%s_%s" % (k[0], k[1]))) for k in P.semkeys}
        with nc.Block() as block:
            P.emit(block, sems)
    return nc


def _consts():
    k = np.arange(128)[:, None]
    q = np.arange(128)[None, :]
    mm = np.zeros((128, 6, 128), np.float32)
    mod4 = ((q - k) % 4 == 0)
    mm[:, 0] = ((q <= k) & mod4)
    mm[:, 1] = mod4
    mm[:, 2] = mod4
    mm[:, 3] = (q <= k).astype(np.float32) + mod4
    mm[:, 4] = (q >= k).astype(np.float32) + ((q >= k) & mod4)
    mm[:, 5] = (q >= k)
    mb = np.where(mm > 1.5, 8.0 * np.log(2.0), np.where(mm > 0.5, 0.0, -BIG)).astype(np.float32)
    m = np.zeros((128, 13, 128), np.float32)
    m[:, 0:6] = mb
    for t_ in range(9, 13):
        m[:, t_] = mb[:, 5]
    invf = (500000.0 ** (-np.arange(0, 16, 2, dtype=np.float32) / 16.0)).astype(np.float32)
    invf = np.broadcast_to(invf[None, :], (128, 8)).copy()
    gb = np.zeros((8, 8), np.float32)
    cb = np.zeros((8, 8), np.float32)
    for b in range(8):
        for n in range(8):
            gb[b, n] = 0.0 if n < b else (1e30 if n == b else -1e30)
            cb[b, n] = -BIG if n > b else 0.0
    gb = np.broadcast_to(np.repeat(gb, 2, axis=0)[None], (128, 16, 8)).copy()
    cb = np.broadcast_to(np.repeat(cb, 2, axis=0)[None], (128, 16, 8)).copy()
    oh = np.zeros((8, 4, S), np.float32)
    for n in range(8):
        oh[n, :, n * 256:(n + 1) * 256] = 1.0
    return m, invf, gb, cb, oh


def make_in_maps(x, c, positions, pre_norm_gain, post_norm_gain, w_ada, b_ada, w_in, w_out, nseq=2, ncores=NCORES):
    m, invf, gb, cb, oh = _consts()
    f = lambda a: np.ascontiguousarray(a, dtype=np.float32)
    pregT = f(pre_norm_gain.reshape(2, 8, 128).transpose(2, 0, 1))
    badaT = f(b_ada[:, :2048].reshape(2, 16, 128).transpose(2, 0, 1))
    badag = f(b_ada[:, 2048:])
    maps = []
    for i in range(ncores):
        sl = slice(i * nseq, (i + 1) * nseq)
        cT = np.zeros((128, 8, 2), np.float32)
        cT[:, :, :nseq] = c[sl].reshape(nseq, 8, 128).transpose(2, 1, 0)
        pos = np.zeros((128, 2, NT), np.int32)
        pos[:, :nseq, :] = positions[sl].reshape(nseq, NT, 128).transpose(2, 0, 1)
        maps.append({
            "x": f(x[sl]), "cT": cT, "pos": pos, "pregT": pregT, "badaT": badaT, "badag": badag,
            "postg": f(post_norm_gain), "w_ada": f(w_ada), "w_in": f(w_in), "w_out": f(w_out),
            "masks": m, "invf": invf, "gbias": gb, "cbias": cb, "onehot": oh,
        })
    return maps


def kernel(x, c, positions, pre_norm_gain, post_norm_gain, w_ada, b_ada, w_in, w_out):
    x = np.asarray(x); c = np.asarray(c); positions = np.asarray(positions)
    maps = make_in_maps(x, c, positions, np.asarray(pre_norm_gain), np.asarray(post_norm_gain),
                        np.asarray(w_ada), np.asarray(b_ada), np.asarray(w_in), np.asarray(w_out))
    nc = build_program(2, 2)
    res = run_bass_kernel_spmd(nc, maps, core_ids=list(range(NCORES)))
    return np.concatenate([np.asarray(r["out"]) for r in res.results], axis=0).astype(np.float32)
```

```python
import contextlib
import os
_SUB = int(os.environ.get('SUB', '99'))
_RSUB = int(os.environ.get('RSUB', '99'))
_ASUB = int(os.environ.get('ASUB', '99'))
_BSUB = int(os.environ.get('BSUB', '99'))
_GSUB = int(os.environ.get('GSUB', '99'))
import math
import numpy as np
import concourse.bass as bass
import concourse.mybir as mybir
from concourse.bass_utils import run_bass_kernel_spmd

F32 = mybir.dt.float32
BF = mybir.dt.bfloat16
I32 = mybir.dt.int32
AF = mybir.ActivationFunctionType
ALU = mybir.AluOpType
AX = mybir.AxisListType

NCORES = 8
S = 2048
D = 1024
NT = 16
BIG = 30000.0
EPS = 1e-6
ENGS = ("sync", "scalar", "vector", "gpsimd", "tensor")


class Op:
    __slots__ = ("eng", "fn", "deps", "is_dma", "semkey", "signal", "sigval")

    def __init__(self, eng, fn, is_dma, semkey):
        self.eng = eng
        self.fn = fn
        self.deps = []
        self.is_dma = is_dma
        self.semkey = semkey
        self.signal = False
        self.sigval = None


class Prog:
    def __init__(self):
        self.q = {e: [] for e in ENGS}
        self.lastw = {}
        self.readers = {}
        self.fence_deps = {}
        self.last_dma = {}

    def op(self, eng, fn, reads=(), writes=(), dma=False, semkey=None, extra=()):
        o = Op(eng, fn, dma, semkey)
        deps = {}
        for k in reads:
            w = self.lastw.get(k)
            if w is not None:
                deps[id(w)] = w
        for k in writes:
            w = self.lastw.get(k)
            if w is not None:
                deps[id(w)] = w
            for r in self.readers.get(k, {}).values():
                deps[id(r)] = r
        for d in extra:
            if d is not None:
                deps[id(d)] = d
        fd = self.fence_deps.pop(eng, None)
        if fd:
            for d in fd:
                deps[id(d)] = d
        o.deps = list(deps.values())
        rk = ("dma", semkey) if dma else eng
        for k in reads:
            self.readers.setdefault(k, {})[rk] = o
        for k in writes:
            self.lastw[k] = o
            self.readers[k] = {}
        self.q[eng].append(o)
        if dma:
            self.last_dma[semkey] = o
        return o

    def fence(self):
        last = [self.q[e][-1] for e in ENGS if self.q[e] and not self.q[e][-1].is_dma]
        last += list(self.last_dma.values())
        for e in ENGS:
            self.fence_deps[e] = list(last)

    def finalize(self):
        for e in ENGS:
            for o in self.q[e]:
                for d in o.deps:
                    if d.eng == "tensor" and e == "tensor" and not d.is_dma and not o.is_dma:
                        continue
                    d.signal = True
        self.semkeys = {}
        for e in ENGS:
            cnt = {}
            for o in self.q[e]:
                if not o.signal:
                    continue
                key = ("dma", o.semkey) if o.is_dma else ("eng", e)
                cnt[key] = cnt.get(key, 0) + (16 if o.is_dma else 1)
                o.sigval = (key, cnt[key])
                self.semkeys[key] = None

    def emit(self, block, sems):
        for e in ENGS:
            ops = self.q[e]
            if not ops:
                continue

            def body(eng, ops=ops, e=e):
                waited = {}
                for o in ops:
                    need = {}
                    for d in o.deps:
                        if d.eng == "tensor" and e == "tensor" and not d.is_dma and not o.is_dma:
                            continue
                        k, v = d.sigval
                        if need.get(k, 0) < v:
                            need[k] = v
                    for k, v in need.items():
                        if waited.get(k, 0) < v:
                            eng.wait_ge(sems[k], v)
                            waited[k] = v
                    if o.fn is None:
                        continue
                    ins = o.fn(eng)
                    if o.signal:
                        ins.then_inc(sems[o.sigval[0]], 16 if o.is_dma else 1)

            getattr(block, e)(body)


def _prod(xs):
    r = 1
    for v in xs:
        r *= v
    return r


def ap(t, off, dims, np_=128, p0=0):
    ps = _prod(list(t.shape)[1:])
    return bass.AP(t, p0 * ps + off, [[ps, np_]] + [list(d) for d in dims])


class _Stop(Exception):
    pass


def build_program(nseq=2, depth=2, dbg=None, stop=None):
    nc = bass.Bass("TRN2", target_bir_lowering=False)
    dt_in = lambda n, s, d=F32: nc.dram_tensor(n, s, d, kind="ExternalInput").ap()
    x_d = dt_in("x", [nseq, S, D])
    cT_d = dt_in("cT", [128, 8, 2])
    pos_d = dt_in("pos", [128, 2, NT], I32)
    pregT_d = dt_in("pregT", [128, 2, 8])
    badaT_d = dt_in("badaT", [128, 2, 16])
    badag_d = dt_in("badag", [2, D])
    postg_d = dt_in("postg", [2, D])
    wada_d = dt_in("w_ada", [2, D, 3 * D])
    win_d = dt_in("w_in", [2, D, 4 * D])
    wout_d = dt_in("w_out", [2, D, D])
    masks_d = dt_in("masks", [128, 13, 128])
    invf_d = dt_in("invf", [128, 8])
    gbias_d = dt_in("gbias", [128, 16, 8])
    cb_d = dt_in("cbias", [128, 16, 8])
    onehot_d = dt_in("onehot", [8, 4, S])
    out_d = nc.dram_tensor("out", [nseq, S, D], F32, kind="ExternalOutput").ap()
    gp_d = nc.dram_tensor("gp_scr", [2, 2, 128, D], F32).ap()
    dbg_d = {}
    if dbg:
        for name, shape in dbg.items():
            dbg_d[name] = nc.dram_tensor("dbg_" + name, shape, F32 if name in ("accA", "accN") else BF, kind="ExternalOutput").ap()

    P = Prog()
    es = contextlib.ExitStack()
    with es:
        sb = lambda n, s, d: es.enter_context(nc.sbuf_tensor("sb_" + n, s, d))
        pp = lambda n, s, d: es.enter_context(nc.psum_tensor("ps_" + n, s, d))
        hT = sb("hT", [128, 8, S], BF)
        OT = sb("OT", [128, 8, S], BF)
        Wb = sb("Wb", [128, 8, 1024], BF)
        QK = sb("QK", [128, 8, S], BF)
        Vt = sb("Vt", [128, NT, 4, 128], BF)
        accA = sb("accA", [128, S], F32)
        ptb = sb("ptb", [128, 4, 640], BF)
        stag = sb("stag", [128, 2, 576], BF)
        augst = sb("augst", [128, 4, 4, 72], BF)
        xt = sb("xt", [128, 4, D], F32)
        xn = sb("xn", [128, 4, D], BF)
        gpb = sb("gpb", [128, D], F32)
        rdt = sb("rdt", [128, 2, 512], F32)
        fT = sb("fT", [128, 2, 512], F32)
        masks = sb("masks", [128, 13, 128], BF)
        ident = sb("ident", [128, 128], BF)
        identf = sb("identf", [128, 128], F32)
        cs2 = sb("cs2", [128, NT, 16], F32)
        sn2 = sb("sn2", [128, NT, 16], F32)
        invf = sb("invf", [128, 8], F32)
        gbias = sb("gbias", [128, 16, 8], F32)
        cbias = sb("cbias", [128, 16, 8], F32)
        cTs = sb("cTs", [128, 8, 2], F32)
        scs = sb("scs", [128, 8, 2], BF)
        pregT = sb("pregT", [128, 2, 8], F32)
        badaT = sb("badaT", [128, 2, 16], F32)
        ABt = sb("ABt", [128, 2, 2, 2, 8], F32)
        tmp8 = sb("tmp8", [128, 8], F32)
        posi = sb("posi", [128, 2, NT], I32)
        rp = [sb("rp%d" % i, [128, NT, 8], F32) for i in range(5)]
        rpi = sb("rpi", [128, NT, 8], I32)
        ropet = sb("ropet", [128, 2, 2, 8, 16], F32)
        rstage = sb("rstage", [128, 2, 8, 16], F32)
        ss4 = sb("ss4", [128, 8], F32)
        rstd4 = sb("rstd4", [128, 8], F32)
        ssy = sb("ssy", [128, 4], F32)
        rsy = sb("rsy", [128, 4], F32)
        junk = sb("junk", [128, D], BF)
        kmf = sb("kmf", [128, 4, 8], F32)
        kmb = sb("kmb", [128, 4, 8], BF)
        g1 = sb("g1", [128, 4, 4, 8], F32)
        cmpt = sb("cmpt", [128, 16, 8, 8], BF)
        rank = sb("rank", [128, 4, 4, 8], F32)
        selt = sb("selt", [128, 4, 4, 8], F32)
        pA = pp("pA", [128, 1024], F32)
        pB = pp("pB", [128, 1024], F32)
        pC = pp("pC", [128, 512], F32)
        pD = pp("pD", [128, 512], F32)
        pT = [pp("pT0", [128, 1024], BF), pp("pT1", [128, 1024], BF)]

        ld = lambda eng, dst, src, key, sk: P.op(eng, lambda e: e.dma_start(out=dst, in_=src),
                                                 writes=[key], dma=True, semkey=sk)
        ld("gpsimd", masks[:], masks_d, "masks", "c0")
        ld("sync", invf[:], invf_d, "invf", "c1")
        ld("sync", gbias[:], gbias_d, "gbias", "c2")
        ld("sync", cbias[:], cb_d, "cbias", "c3")
        ld("sync", cTs[:], cT_d, "cTs", "c4")
        ld("sync", pregT[:], pregT_d, "pregT", "c5")
        ld("sync", badaT[:], badaT_d, "badaT", "c6")
        ld("sync", posi[:], pos_d, "posi", "c7")
        P.op("gpsimd", lambda e: e.memset(identf[:], 0.0), writes=["identf"])
        P.op("gpsimd", lambda e: e.affine_select(out=identf[:], in_=identf[:], pattern=[[1, 128]],
                                                  compare_op=ALU.not_equal, fill=1.0, base=0,
                                                  channel_multiplier=-1),
             reads=["identf"], writes=["identf"])
        P.op("vector", lambda e: e.tensor_copy(out=ident[:], in_=identf[:]), reads=["identf"], writes=["ident"])
        P.op("gpsimd", lambda e: e.memset(Vt[:, :, :, 64:128], 1.0), writes=["Vones"])
        P.op("gpsimd", lambda e: e.memset(augst[:], 0.0), writes=["augst"])

        P.op("scalar", lambda e: e.activation(out=scs[:], in_=cTs[:], func=AF.Silu), reads=["cTs"], writes=["scs"])
        screp_t = xn
        for b in range(nseq):
            P.op("vector", lambda e, b=b: e.tensor_copy(
                out=ap(screp_t, b * 1024, [[128, 8], [1, 128]]),
                in_=ap(scs, b, [[2, 8], [0, 128]])), reads=["scs"], writes=[("screp", b)])
        for l in range(depth):
            ld("sync", accA[:, 0:D], badag_d[l, :].partition_broadcast(128), "accA", "c8")
            ld("sync", accA[:, D:2 * D], postg_d[l, :].partition_broadcast(128), "accA", "c8")
            for g in range(6):
                src = wada_d[l].rearrange("(kc p) c -> p kc c", p=128)[:, :, g * 512:(g + 1) * 512]
                wi = (l * 6 + g) % 2
                Wst = Wb[:, :, wi * 512:(wi + 1) * 512]
                wk = "Wst%d" % wi
                P.op("gpsimd", lambda e, src=src, Wst=Wst: e.dma_start(out=Wst, in_=src), writes=[wk], dma=True,
                     semkey="wst%d" % wi)
                if g < 4:
                    for fc in range(4):
                        Fi = g * 4 + fc
                        for kc in range(8):
                            P.op("tensor", lambda e, Fi=Fi, fc=fc, kc=kc, Wst=Wst: e.matmul(
                                pC[:, Fi * 2:Fi * 2 + nseq], lhsT=Wst[:, kc, fc * 128:(fc + 1) * 128],
                                rhs=scs[:, kc, 0:nseq], start=(kc == 0), stop=(kc == 7)),
                                reads=[wk, "scs"], writes=["pC"])
                else:
                    for b in range(nseq):
                        bank = pA if b == 0 else pB
                        bk = "pA0" if b == 0 else "pB0"
                        for kc in range(8):
                            P.op("tensor", lambda e, b=b, kc=kc, bank=bank, Wst=Wst: e.matmul(
                                bank[:, 0:512],
                                lhsT=ap(screp_t, b * 1024 + kc * 128, [[1, 128]]),
                                rhs=Wst[:, kc, :], start=(kc == 0), stop=(kc == 7)),
                                reads=[wk, ("screp", b)], writes=[bk])
                        c0 = (g - 4) * 512
                        gt = gpb[:, b * 512:(b + 1) * 512]
                        P.op("vector", lambda e, bank=bank, c0=c0, gt=gt: e.tensor_tensor(
                            out=gt, in0=bank[:, 0:512], in1=accA[:, c0:c0 + 512], op=ALU.add),
                            reads=[bk, "accA"], writes=[("gpb", b)])
                        P.op("vector", lambda e, c0=c0, gt=gt: e.tensor_tensor(
                            out=gt, in0=gt, in1=accA[:, D + c0:D + c0 + 512], op=ALU.mult),
                            reads=[("gpb", b), "accA"], writes=[("gpb", b)])
                        P.op("sync", lambda e, l=l, b=b, c0=c0, gt=gt: e.dma_start(
                            out=gp_d[l, b, :, c0:c0 + 512], in_=gt),
                            reads=[("gpb", b)], writes=[("gpd", l, b)], dma=True, semkey="gpst%d" % b)
            for b in range(nseq):
                P.op("vector", lambda e, l=l, b=b: e.tensor_tensor(
                    out=ABt[:, l, b, 1, :], in0=ap(pC, b, [[2, 8]]), in1=badaT[:, l, 0:8], op=ALU.add),
                    reads=["pC", "badaT"], writes=[("ABt", l, b)])
                P.op("vector", lambda e, l=l, b=b: e.tensor_tensor(
                    out=tmp8[:], in0=ap(pC, 16 + b, [[2, 8]]), in1=badaT[:, l, 8:16], op=ALU.add),
                    reads=["pC", "badaT"], writes=["tmp8"])
                P.op("vector", lambda e, l=l, b=b: e.scalar_tensor_tensor(
                    out=ABt[:, l, b, 0, :], in0=tmp8[:], scalar=1.0, in1=pregT[:, l, :],
                    op0=ALU.add, op1=ALU.mult),
                    reads=["tmp8", "pregT"], writes=[("ABt", l, b)])
        P.fence()

        TWO_PI = 2.0 * math.pi
        C1 = 6.28125
        C2 = TWO_PI - C1

        def rope_tables(b):
            posf, ang, a2, kf, r = rp
            vop = lambda fn, reads, writes: P.op("vector", fn, reads=reads, writes=writes)
            vop(lambda e: e.tensor_copy(out=ang[:, :, 0], in_=posi[:, b, :]), ["posi"], ["r_posf"])
            vop(lambda e: e.tensor_tensor(out=posf[:], in0=ap(ang, 0, [[8, NT], [0, 8]]),
                                          in1=ap(invf, 0, [[0, NT], [1, 8]]), op=ALU.mult),
                ["r_posf", "invf"], ["r_ang"])
            for which, shift in ((0, 0.0), (1, 0.5 * math.pi)):
                vop(lambda e, shift=shift: e.tensor_scalar(out=a2[:], in0=posf[:], scalar1=shift, scalar2=None,
                                                           op0=ALU.add), ["r_ang"], ["r_a2"])
                vop(lambda e: e.tensor_scalar(out=rpi[:], in0=a2[:], scalar1=1.0 / TWO_PI, scalar2=None,
                                              op0=ALU.mult), ["r_a2"], ["r_ki"])
                vop(lambda e: e.tensor_copy(out=kf[:], in_=rpi[:]), ["r_ki"], ["r_kf"])
                vop(lambda e: e.scalar_tensor_tensor(out=r[:], in0=kf[:], scalar=-C1, in1=a2[:],
                                                     op0=ALU.mult, op1=ALU.add), ["r_kf", "r_a2"], ["r_r"])
                vop(lambda e: e.scalar_tensor_tensor(out=r[:], in0=kf[:], scalar=-C2, in1=r[:],
                                                     op0=ALU.mult, op1=ALU.add), ["r_kf", "r_r"], ["r_r"])
                vop(lambda e: e.tensor_scalar(out=kf[:], in0=r[:], scalar1=math.pi, scalar2=-TWO_PI,
                                              op0=ALU.is_gt, op1=ALU.mult), ["r_r"], ["r_kf"])
                vop(lambda e: e.tensor_tensor(out=r[:], in0=r[:], in1=kf[:], op=ALU.add), ["r_kf", "r_r"], ["r_r"])
                vop(lambda e: e.tensor_scalar(out=kf[:], in0=r[:], scalar1=-math.pi, scalar2=TWO_PI,
                                              op0=ALU.is_lt, op1=ALU.mult), ["r_r"], ["r_kf"])
                vop(lambda e: e.tensor_tensor(out=r[:], in0=r[:], in1=kf[:], op=ALU.add), ["r_kf", "r_r"], ["r_r"])
                vop(lambda e: e.tensor_scalar(out=r[:], in0=r[:], scalar1=3.1415925, scalar2=-3.1415925,
                                              op0=ALU.min, op1=ALU.max), ["r_r"], ["r_r"])
                if which == 0:
                    P.op("scalar", lambda e: e.activation(out=sn2[:, :, 8:16], in_=r[:], func=AF.Sin),
                         reads=["r_r"], writes=["sn2"])
                    P.op("scalar", lambda e: e.mul(out=sn2[:, :, 0:8], in_=sn2[:, :, 8:16], mul=-1.0),
                         reads=["sn2"], writes=["sn2"])
                else:
                    P.op("scalar", lambda e: e.activation(out=cs2[:, :, 0:8], in_=r[:], func=AF.Sin),
                         reads=["r_r"], writes=["cs2"])
                    P.op("scalar", lambda e: e.copy(out=cs2[:, :, 8:16], in_=cs2[:, :, 0:8]),
                         reads=["cs2"], writes=["cs2"])

        banks = {"pA0": pA[:, 0:512], "pA1": pA[:, 512:1024], "pB0": pB[:, 0:512], "pB1": pB[:, 512:1024],
                 "pC": pC[:], "pD": pD[:]}

        def dump(name, src_ap, key):
            if name in dbg_d:
                P.op("sync", lambda e: e.dma_start(out=dbg_d[name], in_=src_ap), reads=key, dma=True,
                     semkey="dbg_" + name, writes=[("dbgout", name)])

        def load_wslice(l, mixer, hf):
            base = mixer * 2048
            wv = win_d[l].rearrange("(kc p) c -> p kc c", p=128)
            for i in range(4):
                c0 = base + i * 512 + hf * 256
                P.op("gpsimd", lambda e, i=i, c0=c0: e.dma_start(out=Wb[:, :, i * 256:(i + 1) * 256],
                                                                   in_=wv[:, :, c0:c0 + 256]),
                     writes=[("Wb", i)], dma=True, semkey="wb%d" % i)

        def norm_phase(b, l, xsrc):
            for hg in range(8):
                par = hg % 2
                for t2 in range(2):
                    sl = par * 2 + t2
                    tt_ = hg * 2 + t2
                    P.op("sync", lambda e, sl=sl, tt_=tt_: e.dma_start(out=xt[:, sl, :], in_=xsrc[b, tt_ * 128:(tt_ + 1) * 128, :]),
                         reads=[("xd", b, tt_)], writes=[("xt", sl)], dma=True, semkey="xt%d" % sl)
                for t2 in range(2):
                    sl = par * 2 + t2
                    P.op("scalar", lambda e, sl=sl: e.activation(out=junk[:], in_=xt[:, sl, :], func=AF.Square,
                                                                  accum_out=ss4[:, sl:sl + 1]),
                         reads=[("xt", sl)], writes=["junk", ("ss4", par)])
                P.op("scalar", lambda e, par=par: e.activation(out=ss4[:, 4 + 2 * par:6 + 2 * par], in_=ss4[:, 2 * par:2 * par + 2],
                                                                func=AF.Sqrt, bias=EPS_AP, scale=1.0 / D),
                     reads=[("ss4", par)], writes=[("ss4b", par)])
                P.op("vector", lambda e, par=par: e.reciprocal(out=rstd4[:, 2 * par:2 * par + 2], in_=ss4[:, 4 + 2 * par:6 + 2 * par]),
                     reads=[("ss4b", par)], writes=[("rstd4", par)])
                for t2 in range(2):
                    sl = par * 2 + t2
                    P.op("vector", lambda e, sl=sl: e.tensor_scalar(out=xn[:, sl, :], in0=xt[:, sl, :],
                                                                     scalar1=rstd4[:, sl:sl + 1], scalar2=None, op0=ALU.mult),
                         reads=[("xt", sl), ("rstd4", par)], writes=[("xn", sl)])
                for c in range(8):
                    pt_ = pT[c % 2]
                    pk = ("pT", c % 2)
                    for t2 in range(2):
                        sl = par * 2 + t2
                        P.op("tensor", lambda e, c=c, t2=t2, sl=sl, pt_=pt_: e.transpose(
                            pt_[:, t2 * 128:(t2 + 1) * 128], xn[:, sl, c * 128:(c + 1) * 128], ident[:]),
                            reads=[("xn", sl), "ident"], writes=[pk])
                    P.op("vector", lambda e, c=c, pt_=pt_, hg=hg: e.tensor_scalar(
                        out=hT[:, c, hg * 256:(hg + 1) * 256], in0=pt_[:, 0:256],
                        scalar1=ABt[:, l, b, 0, c:c + 1], scalar2=ABt[:, l, b, 1, c:c + 1],
                        op0=ALU.mult, op1=ALU.add),
                        reads=[pk, ("ABt", l, b)], writes=[("hT", hg // 2)])

        def proj_tile_mm(tt, mixer):
            qb, qk_ = (pA[:, 0:512], "pA0") if tt % 2 == 0 else (pA[:, 512:1024], "pA1")
            vb, vk = (pB[:, 0:512], "pB0") if tt % 2 == 0 else (pB[:, 512:1024], "pB1")
            for kc in range(8):
                P.op("tensor", lambda e, kc=kc: e.matmul(qb, lhsT=hT[:, kc, tt * 128:(tt + 1) * 128],
                                                          rhs=Wb[:, kc, 0:512], start=(kc == 0), stop=(kc == 7)),
                     reads=[("hT", tt // 4), ("Wb", 0), ("Wb", 1)], writes=[qk_])
            for kc in range(8):
                P.op("tensor", lambda e, kc=kc: e.matmul(vb[:, 0:256], lhsT=hT[:, kc, tt * 128:(tt + 1) * 128],
                                                          rhs=Wb[:, kc, 512:768], start=(kc == 0), stop=(kc == 7)),
                     reads=[("hT", tt // 4), ("Wb", 2)], writes=[vk])
            return qb, qk_, vb, vk

        def proj_tile_post(tt, mixer, qb, qk_, vb, vk):
            W = 64 if mixer == 0 else 72
            if _SUB < 1:
                return
            sbuf_i = tt % 2
            sk = ("stag", sbuf_i)
            st = lambda off, dims: ap(stag, sbuf_i * 576 + off, dims)
            psv = lambda off, dims: bass.AP(qb.tensor, qb.offset + off, [list(qb.ap[0])] + [list(d) for d in dims])
            P.op("scalar", lambda e: e.activation(out=st(0, [[W, 8], [1, 64]]), in_=psv(0, [[64, 8], [1, 64]]),
                                                  func=AF.Copy), reads=[qk_], writes=[sk])
            if _SUB < 2:
                return
            ra = lambda off, dims: ap(ropet, sbuf_i * 256 + off, dims)
            rk = ("ropet", sbuf_i)
            rsk = ("rstage", sbuf_i)
            rs_ = lambda off, dims: ap(rstage, sbuf_i * 128 + off, dims)
            P.op("scalar", lambda e: e.activation(out=rs_(0, [[16, 8], [1, 16]]), in_=psv(0, [[64, 8], [1, 16]]),
                                                  func=AF.Copy), reads=[qk_], writes=[rsk])
            P.op("vector", lambda e: e.tensor_tensor(out=ra(0, [[16, 8], [1, 16]]), in0=rs_(0, [[16, 8], [1, 16]]),
                                                     in1=ap(cs2, tt * 16, [[0, 8], [1, 16]]), op=ALU.mult),
                 reads=[rsk, "cs2"], writes=[rk])
            if _RSUB < 2:
                return
            P.op("vector", lambda e: e.tensor_tensor(out=ra(128, [[16, 8], [1, 8]]), in0=rs_(8, [[16, 8], [1, 8]]),
                                                     in1=ap(sn2, tt * 16, [[0, 8], [1, 8]]), op=ALU.mult),
                 reads=[rsk, "sn2"], writes=[rk])
            P.op("vector", lambda e: e.tensor_tensor(out=ra(128 + 8, [[16, 8], [1, 8]]), in0=rs_(0, [[16, 8], [1, 8]]),
                                                     in1=ap(sn2, tt * 16 + 8, [[0, 8], [1, 8]]), op=ALU.mult),
                 reads=[rsk, "sn2"], writes=[rk])
            if _RSUB < 3:
                return
            P.op("vector", lambda e: e.tensor_tensor(out=st(0, [[W, 8], [1, 16]]), in0=ra(0, [[16, 8], [1, 16]]),
                                                     in1=ra(128, [[16, 8], [1, 16]]), op=ALU.add),
                 reads=[rk, sk], writes=[sk])
            if _SUB < 3:
                return
            vps = bass.AP(vb.tensor, vb.offset, [list(vb.ap[0]), [64, 4], [1, 64]])
            P.op("scalar", lambda e: e.activation(out=Vt[:, tt, :, 0:64], in_=vps, func=AF.Copy),
                 reads=[vk], writes=[("V", tt)])
            if _SUB < 4:
                return
            pt_ = pT[tt % 2]
            pk = ("pT", tt % 2)
            if mixer == 0:
                for i in range(4):
                    P.op("tensor", lambda e, i=i: e.transpose(pt_[:, i * 128:(i + 1) * 128],
                                                              st(i * 128, [[1, 128]]), ident[:]),
                         reads=[sk, "ident"], writes=[pk])
                P.op("vector", lambda e: e.tensor_copy(out=ap(QK, tt * 128, [[S, 4], [1, 128]]),
                                                       in_=ap(pt_, 0, [[128, 4], [1, 128]])),
                     reads=[pk], writes=[("QK", s_, tt) for s_ in range(4)])
            else:
                for i in range(8):
                    P.op("tensor", lambda e, i=i: e.transpose(pt_[0:64, i * 128:(i + 1) * 128],
                                                              st(i * 72, [[1, 64]]), ident[:]),
                         reads=[sk, "ident"], writes=[pk])
                P.op("vector", lambda e: e.tensor_copy(out=ap(QK, tt * 128, [[S, 8], [1, 128]], np_=64),
                                                       in_=ap(pt_, 0, [[128, 8], [1, 128]], np_=64)),
                     reads=[pk], writes=[("QK", s_, tt) for s_ in range(8)] + ["QKhi_A"])

        def proj_phase(b, l, mixer, hf):
            prev = None
            for tt in range(NT):
                cur = proj_tile_mm(tt, mixer)
                if prev is not None:
                    proj_tile_post(tt - 1, mixer, *prev)
                prev = cur
            proj_tile_post(NT - 1, mixer, *prev)
            if _SUB < 5:
                return
            for pr in range(2):
                ch = mixer * 4 + hf * 2 + pr
                for tc in range(4):
                    bank, bk = (pC[:], "pC") if (pr * 4 + tc) % 2 == 0 else (pD[:], "pD")
                    for kc in range(8):
                        P.op("tensor", lambda e, kc=kc, pr=pr, tc=tc, bank=bank: e.matmul(
                            bank, lhsT=Wb[:, kc, 768 + pr * 128:768 + (pr + 1) * 128],
                            rhs=hT[:, kc, tc * 512:(tc + 1) * 512], start=(kc == 0), stop=(kc == 7)),
                            reads=[("hT", tc), ("Wb", 3)], writes=[bk])
                    P.op("scalar", lambda e, ch=ch, tc=tc, bank=bank: e.activation(
                        out=OT[:, ch, tc * 512:(tc + 1) * 512], in_=bank, func=AF.Silu),
                        reads=[bk], writes=[("OT", ch, tc)])
            if _SUB < 6:
                return
            if mixer == 0:
                P.op("gpsimd", lambda e: e.memset(ap(QK, 4 * S + 64, [[128, 64], [1, 64]]), 1.0),
                     writes=[("QK", s_, t_) for s_ in range(4, 8) for t_ in range(NT)] + ["QKhi_A"])
                for r in range(16):
                    bank, bk = (pC[:], "pC") if r % 2 == 0 else (pD[:], "pD")
                    for kc in range(8):
                        P.op("tensor", lambda e, kc=kc, r=r, bank=bank: e.matmul(
                            bank[:, 0:256], lhsT=ap(hT, kc * S + r, [[16, 128]]), rhs=Wb[:, kc, 512:768],
                            start=(kc == 0), stop=(kc == 7)),
                            reads=[("hT", 0), ("hT", 1), ("hT", 2), ("hT", 3), ("Wb", 2)], writes=[bk])
                    P.op("vector", lambda e, r=r, bank=bank: e.tensor_copy(
                        out=ap(QK, 4 * S + r * 512, [[128, 4], [1, 64]]),
                        in_=bass.AP(bank.tensor, bank.offset, [list(bank.ap[0]), [64, 4], [1, 64]])),
                        reads=[bk], writes=[("Vp", r)], extra=[P.lastw.get(("QK", 4, 0))])

        def normalize(j, src_bank, src_key, ncols, ch, col0, which):
            pb = (j % 2) * 64
            db = 64 - pb
            rd = ap(rdt, which * 512, [[1, ncols]], np_=64, p0=pb)
            ff = ap(fT, which * 512, [[1, ncols]], np_=64, p0=pb)
            srcn = bass.AP(src_bank.tensor, src_bank.offset + pb * src_bank.ap[0][0], [[src_bank.ap[0][0], 64], [1, ncols]])
            srcd = bass.AP(src_bank.tensor, src_bank.offset + db * src_bank.ap[0][0], [[src_bank.ap[0][0], 64], [1, ncols]])
            P.op("vector", lambda e: e.reciprocal(out=rd, in_=srcd), reads=[src_key], writes=[("rdt", which)])
            P.op("vector", lambda e: e.tensor_tensor(out=ff, in0=srcn, in1=rd, op=ALU.mult),
                 reads=[src_key, ("rdt", which)], writes=[("fT", which)])
            ot = ap(OT, ch * S + col0, [[1, ncols]], np_=64, p0=pb)
            P.op("gpsimd", lambda e: e.tensor_tensor(out=ot, in0=ot, in1=ff, op=ALU.mult),
                 reads=[("fT", which)] + [("OT", ch, tc) for tc in range(col0 // 512, (col0 + ncols - 1) // 512 + 1)],
                 writes=[("OT", ch, tc) for tc in range(col0 // 512, (col0 + ncols - 1) // 512 + 1)])

        def vaug(src, tile_off, j):
            off = tile_off + j * 128 - (64 if j % 2 else 0)
            return ap(src, off, [[1, 128]])

        def attn_A(b, l, hf):
            ptn = [0]

            def head(j):
                pr = j // 2
                pb = (j % 2) * 64
                ch = hf * 2 + pr
                qT = lambda c0, n, step=1: ap(QK, pr * S + c0, [[step, n]], np_=64, p0=pb)
                kT = lambda c0, n, step=1: ap(QK, (2 + pr) * S + c0, [[step, n]], np_=64, p0=pb)
                qkeys = lambda ts: [("QK", pr, t) for t in ts]
                kkeys = lambda ts: [("QK", 2 + pr, t) for t in ts]
                units = []
                for i in range(NT):
                    kts = list(range(max(0, i - 4), i + 1))
                    units.append((i, kts))

                def score(u, ui):
                    i, kts = u
                    sb_, keys = (pA, ["pA0", "pA1"]) if ui % 2 == 0 else (pB, ["pB0", "pB1"])
                    n = len(kts)
                    n0 = min(n, 4)
                    P.op("tensor", lambda e: e.matmul(sb_[:, 0:n0 * 128], lhsT=ident[:],
                                                      rhs=ap(masks, (5 - n) * 128, [[1, n0 * 128]]), start=True, stop=False),
                         reads=["ident", "masks"], writes=[keys[0]])
                    if n == 5:
                        P.op("tensor", lambda e: e.matmul(sb_[:, 512:640], lhsT=ident[:],
                                                          rhs=ap(masks, 4 * 128, [[1, 128]]), start=True, stop=False),
                             reads=["ident", "masks"], writes=[keys[1]])
                    for s_, kt in enumerate(kts):
                        P.op("tensor", lambda e, s_=s_, kt=kt: e.matmul(
                            sb_[:, s_ * 128:(s_ + 1) * 128], lhsT=kT(kt * 128, 128), rhs=qT(i * 128, 128),
                            start=False, stop=(s_ == n0 - 1 or s_ == 4)),
                            reads=qkeys([i]) + kkeys([kt]), writes=[keys[s_ // 4]])
                    return sb_, keys

                def softmax_pv(u, ui, sb_, keys):
                    i, kts = u
                    n = len(kts)
                    pi = ptn[0] % 3
                    ptn[0] += 1
                    pt_ = ap(ptb, pi * 640, [[1, n * 128]])
                    P.op("scalar", lambda e: e.activation(out=pt_, in_=sb_[:, 0:n * 128], func=AF.Exp, scale=0.125),
                         reads=keys if n > 4 else keys[:1], writes=[("pt", pi)])
                    if dbg and j == 0 and hf == 0 and b == 0 and l == 0 and i in (0, 5):
                        dump("pt%d" % i, ptb[:, pi, :], [("pt", pi)])
                    accb, ak = (pC, "pC") if (i // 4) % 2 == 0 else (pD, "pD")
                    for s_, kt in enumerate(kts):
                        P.op("tensor", lambda e, s_=s_, kt=kt: e.matmul(
                            accb[:, (i % 4) * 128:(i % 4 + 1) * 128], lhsT=vaug(Vt, kt * 512, j),
                            rhs=ap(ptb, pi * 640 + s_ * 128, [[1, 128]]), start=(s_ == 0), stop=(s_ == n - 1)),
                            reads=[("V", kt), "Vones", ("pt", pi)], writes=[ak])
                    if i % 4 == 3:
                        c0 = (i // 4) * 512
                        P.op("scalar", lambda e: e.activation(out=accA[:, c0:c0 + 512], in_=accb[:], func=AF.Copy),
                             reads=[ak], writes=[("accA", i // 4)])
                        if dbg and j == 0 and hf == 0 and b == 0 and l == 0 and i == 15:
                            dump("accN", accA[:], [("accA", q_) for q_ in range(4)])

                pend = score(units[0], 0)
                for ui in range(len(units)):
                    nxt = score(units[ui + 1], ui + 1) if ui + 1 < len(units) else None
                    softmax_pv(units[ui], ui, *pend)
                    pend = nxt
                if _ASUB < 1 and hf == 1:
                    return
                def score3(g):
                    sb_, key = (pA[:, 0:512], "pA0") if g % 2 == 0 else (pB[:, 0:512], "pB0")
                    P.op("tensor", lambda e: e.matmul(sb_, lhsT=ident[:], rhs=ap(masks, 9 * 128, [[1, 512]]),
                                                      start=True, stop=False),
                         reads=["ident", "masks"], writes=[key])
                    for s_ in range(4):
                        r = 4 * g + s_
                        P.op("tensor", lambda e, s_=s_, r=r: e.matmul(
                            sb_[:, s_ * 128:(s_ + 1) * 128], lhsT=kT(r, 128, 16), rhs=qT(r, 128, 16),
                            start=False, stop=(s_ == 3)),
                            reads=qkeys(range(NT)) + kkeys(range(NT)), writes=[key])
                    return sb_, key

                def pv3(g, sb_, key):
                    pi = ptn[0] % 3
                    ptn[0] += 1
                    pt_ = ap(ptb, pi * 640, [[1, 512]])
                    P.op("scalar", lambda e: e.activation(out=pt_, in_=sb_, func=AF.Exp, scale=0.125),
                         reads=[key], writes=[("pt", pi)])
                    accb, ak = (pC, "pC") if g % 2 == 0 else (pD, "pD")
                    for s_ in range(4):
                        r = 4 * g + s_
                        P.op("tensor", lambda e, s_=s_, r=r: e.matmul(
                            accb[:, s_ * 128:(s_ + 1) * 128], lhsT=vaug(QK, 4 * S + r * 512, j),
                            rhs=ap(ptb, pi * 640 + s_ * 128, [[1, 128]]), start=True, stop=True),
                            reads=[("Vp", r), ("pt", pi), "QKhi_A"], writes=[ak])
                    P.op("vector", lambda e: e.tensor_tensor(
                        out=ap(accA, 4 * g, [[1, 4], [16, 128]]), in0=ap(accA, 4 * g, [[1, 4], [16, 128]]),
                        in1=ap(accb, 0, [[128, 4], [1, 128]]), op=ALU.add),
                        reads=[ak] + [("accA", q_) for q_ in range(4)], writes=[("accA", q_) for q_ in range(4)])

                pend = score3(0)
                for g in range(4):
                    nxt = score3(g + 1) if g + 1 < 4 else None
                    pv3(g, *pend)
                    pend = nxt
                if dbg and j == 0 and hf == 0 and b == 0 and l == 0:
                    dump("accA", accA[:], [("accA", q_) for q_ in range(4)])
                if _ASUB < 2 and hf == 1:
                    return
                for q_ in range(4):
                    normalize(j, accA[:, q_ * 512:(q_ + 1) * 512], ("accA", q_), 512, ch, q_ * 512, q_ % 2)

            for j in range(4):
                head(j)

        def attn_B(b, l, hf):
            P.op("gpsimd", lambda e: e.dma_start(out=ap(QK, 4 * S, [[S, 4], [1, S]], np_=8, p0=64), in_=onehot_d),
                 writes=[("QKaugK",)] , dma=True, semkey="onehot",
                 extra=[P.lastw.get(("QK", s_, t_)) for s_ in range(4, 8) for t_ in (0, NT - 1)])
            if _BSUB < 1:
                return
            for j in range(4):
                P.op("vector", lambda e, j=j: e.tensor_reduce(
                    out=kmf[0:64, j, :], in_=ap(QK, (4 + j) * S, [[256, 8], [1, 256]], np_=64), axis=AX.X, op=ALU.add),
                    reads=[("QK", 4 + j, t_) for t_ in range(NT)], writes=["kmf"])
            P.op("vector", lambda e: e.tensor_scalar(out=kmb[0:64, :, :], in0=kmf[0:64, :, :], scalar1=1.0 / 256,
                                                     scalar2=None, op0=ALU.mult), reads=["kmf"], writes=["kmb"])
            if _BSUB < 2:
                return
            for tt in range(NT):
                for j in range(4):
                    P.op("tensor", lambda e, j=j, tt=tt: e.matmul(pC[:, tt * 32 + j * 8:tt * 32 + (j + 1) * 8],
                                                                  lhsT=ap(QK, j * S + tt * 128, [[1, 128]], np_=64),
                                                                  rhs=kmb[0:64, j, :], start=True, stop=True),
                         reads=[("QK", j, tt), "kmb"], writes=["pC"])
            P.op("tensor", lambda e: e.matmul(pD[:, 0:128], lhsT=ident[:], rhs=ident[:], start=True, stop=True),
                 reads=["ident"], writes=["pC", "pD"])
            gall = fT[:, 0, :]
            P.op("vector", lambda e: e.tensor_copy(out=gall, in_=pC[:]), reads=["pC"], writes=[("fT", 0)])
            def gate_group(gq):
                t0_ = 4 * gq
                g1v = ap(g1, 0, [[1, 128]])
                P.op("vector", lambda e: e.tensor_tensor(out=ap(g1, 0, [[32, 4], [8, 4], [1, 8]]),
                                                         in0=ap(fT, t0_ * 32, [[32, 4], [8, 4], [1, 8]]),
                                                         in1=ap(gbias, t0_ * 8, [[8, 4], [0, 4], [1, 8]]), op=ALU.add),
                     reads=[("fT", 0), "gbias"], writes=["g1"])
                P.op("vector", lambda e: e.tensor_tensor(
                    out=ap(cmpt, 0, [[64, 16], [8, 8], [1, 8]]),
                    in0=ap(g1, 0, [[8, 16], [0, 8], [1, 8]]), in1=ap(g1, 0, [[8, 16], [1, 8], [0, 8]]),
                    op=ALU.is_gt), reads=["g1"], writes=["cmpt"])
                P.op("vector", lambda e: e.tensor_reduce(out=ap(rank, 0, [[8, 16], [1, 8]]),
                                                         in_=ap(cmpt, 0, [[64, 16], [8, 8], [1, 8]]),
                                                         axis=AX.X, op=ALU.add),
                     reads=["cmpt"], writes=["rank"])
                P.op("vector", lambda e: e.tensor_scalar(out=ap(selt, 0, [[1, 128]]), in0=ap(rank, 0, [[1, 128]]),
                                                         scalar1=3.5, scalar2=-BIG, op0=ALU.is_gt, op1=ALU.mult),
                     reads=["rank"], writes=["selt"])
                P.op("vector", lambda e: e.tensor_tensor(out=ap(augst, 64, [[288, 4], [72, 4], [1, 8]]),
                                                         in0=ap(selt, 0, [[32, 4], [8, 4], [1, 8]]),
                                                         in1=ap(cbias, t0_ * 8, [[8, 4], [0, 4], [1, 8]]), op=ALU.add),
                     reads=["selt", "cbias"], writes=["augst"])
                for half in range(2):
                    pt_ = pT[half]
                    pk = ("pT", half)
                    for tl in range(2):
                        for j in range(4):
                            P.op("tensor", lambda e, j=j, tl=tl, pt_=pt_, half=half: e.transpose(
                                pt_[0:72, (tl * 4 + j) * 128:(tl * 4 + j + 1) * 128],
                                ap(augst, (half * 2 + tl) * 288 + j * 72, [[1, 72]]), ident[:]),
                                reads=["augst", "ident"], writes=[pk])
                    tt0 = t0_ + half * 2
                    P.op("vector", lambda e, pt_=pt_, tt0=tt0: e.tensor_copy(
                        out=ap(QK, tt0 * 128, [[128, 2], [S, 4], [1, 128]], np_=8, p0=64),
                        in_=ap(pt_, 0, [[512, 2], [128, 4], [1, 128]], np_=8, p0=64)),
                        reads=[pk], writes=[("QKaugQ", tt0), ("QKaugQ", tt0 + 1)])

            for gq in range(4):
                gate_group(gq)
            if dbg and hf == 0 and b == 0 and l == 0:
                dump("qkB", QK[:], [("QKaugQ", t_) for t_ in range(NT)] + [("QKaugK",)])
            if _BSUB < 3:
                return
            ptn = [0]
            sbanks = [(pA[:, 0:512], "pA0"), (pA[:, 512:1024], "pA1"), (pB[:, 0:512], "pB0"), (pB[:, 512:1024], "pB1")]
            def head(j):
                pb = (j % 2) * 64
                ch = 4 + hf * 2 + j // 2
                units = []
                for c in range(4):
                    for kt in range(4 * c + 4):
                        units.append((c, kt))

                def score(u, ui):
                    c, kt = u
                    col0 = max(kt, 4 * c) * 128
                    ncols = (4 * c + 4) * 128 - col0
                    sb_, key = sbanks[ui % 4]
                    qts = list(range(col0 // 128, 4 * c + 4))
                    diag = kt >= 4 * c
                    if diag:
                        P.op("tensor", lambda e: e.matmul(
                            bass.AP(sb_.tensor, sb_.offset, [list(sb_.ap[0]), [1, ncols]]),
                            lhsT=ident[:], rhs=ap(masks, 5 * 128, [[1, ncols]]), start=True, stop=False),
                            reads=["ident", "masks"], writes=[key])
                    P.op("tensor", lambda e: e.matmul(
                        bass.AP(sb_.tensor, sb_.offset, [list(sb_.ap[0]), [1, ncols]]),
                        lhsT=ap(QK, (4 + j) * S + kt * 128, [[1, 128]], np_=72),
                        rhs=ap(QK, j * S + col0, [[1, ncols]], np_=72), start=(not diag), stop=True),
                        reads=[("QK", j, t_) for t_ in qts] + [("QKaugQ", t_) for t_ in qts]
                        + [("QK", 4 + j, kt), ("QKaugK",)], writes=[key])
                    return sb_, key, col0, ncols

                def pv(u, ui, sb_, key, col0, ncols):
                    c, kt = u
                    pi = ui % 4
                    pt_ = ap(ptb, pi * 640, [[1, ncols]])
                    P.op("scalar", lambda e: e.activation(
                        out=pt_, in_=bass.AP(sb_.tensor, sb_.offset, [list(sb_.ap[0]), [1, ncols]]),
                        func=AF.Exp, scale=0.125), reads=[key], writes=[("pt", pi)])
                    accb, ak = (pC, "pC") if c % 2 == 0 else (pD, "pD")
                    oc = col0 - 4 * c * 128
                    P.op("tensor", lambda e: e.matmul(accb[:, oc:oc + ncols], lhsT=vaug(Vt, kt * 512, j), rhs=pt_,
                                                      start=(kt == 0), stop=(kt == 4 * c + 3)),
                         reads=[("V", kt), "Vones", ("pt", pi)], writes=[ak])
                    if kt == 4 * c + 3:
                        normalize(j, accb[:], ak, 512, ch, c * 512, c % 2)

                DEPTHP = 3
                pend = []
                for ui in range(min(DEPTHP, len(units))):
                    pend.append(score(units[ui], ui))
                for ui in range(len(units)):
                    cur = pend.pop(0)
                    pv(units[ui], ui, *cur)
                    if ui + DEPTHP < len(units):
                        pend.append(score(units[ui + DEPTHP], ui + DEPTHP))

            for j in range(4):
                head(j)

        def load_wout(l):
            wv = wout_d[l].rearrange("(kc p) c -> p kc c", p=128)
            for i in range(2):
                P.op("gpsimd", lambda e, i=i: e.dma_start(out=Wb[:, :, i * 512:(i + 1) * 512],
                                                            in_=wv[:, :, i * 512:(i + 1) * 512]),
                     writes=[("Wb", 2 * i), ("Wb", 2 * i + 1)], dma=True, semkey="wb%d" % (2 * i))

        def out_phase(b, l, xsrc):
            P.op("sync", lambda e: e.dma_start(out=gpb[:], in_=gp_d[l, b]), reads=[("gpd", l, b)],
                 writes=[("gpb", 0), ("gpb", 1)], dma=True, semkey="gpld")
            def xload(t_):
                xs_ = t_ % 2
                P.op("sync", lambda e: e.dma_start(out=xt[:, xs_, :], in_=xsrc[b, t_ * 128:(t_ + 1) * 128, :]),
                     reads=[("xd", b, t_)], writes=[("xt", xs_)], dma=True, semkey="xo%d" % xs_)

            xload(0)
            for tt in range(NT):
                yb, yk = (pA, ["pA0", "pA1"]) if tt % 2 == 0 else (pB, ["pB0", "pB1"])
                xs = tt % 2
                ts = 2 + tt % 2
                if tt + 1 < NT:
                    xload(tt + 1)
                for nb in range(2):
                    for c in range(8):
                        P.op("tensor", lambda e, nb=nb, c=c, yb=yb, tt=tt: e.matmul(
                            yb[:, nb * 512:(nb + 1) * 512], lhsT=OT[:, c, tt * 128:(tt + 1) * 128],
                            rhs=Wb[:, c, nb * 512:(nb + 1) * 512], start=(c == 0), stop=(c == 7)),
                            reads=[("OT", c, tt // 4), ("Wb", 2 * nb), ("Wb", 2 * nb + 1)], writes=[yk[nb]])
                P.op("scalar", lambda e, yb=yb, xs=xs: e.activation(out=junk[:], in_=yb[:], func=AF.Square,
                                                                     accum_out=ssy[:, xs:xs + 1]),
                     reads=yk, writes=["junk", ("ssy", xs)])
                P.op("scalar", lambda e, xs=xs: e.activation(out=ssy[:, 2 + xs:3 + xs], in_=ssy[:, xs:xs + 1], func=AF.Sqrt,
                                                              bias=EPS_AP, scale=1.0 / D),
                     reads=[("ssy", xs)], writes=[("ssyb", xs)])
                P.op("vector", lambda e, xs=xs: e.reciprocal(out=rsy[:, xs:xs + 1], in_=ssy[:, 2 + xs:3 + xs]),
                     reads=[("ssyb", xs)], writes=[("rsy", xs)])
                P.op("vector", lambda e, yb=yb, xs=xs, ts=ts: e.scalar_tensor_tensor(
                    out=xt[:, ts, :], in0=yb[:], scalar=rsy[:, xs:xs + 1], in1=gpb[:], op0=ALU.mult, op1=ALU.mult),
                    reads=yk + [("rsy", xs), ("gpb", 0), ("gpb", 1)], writes=[("xt", ts)])
                P.op("gpsimd", lambda e, xs=xs, ts=ts: e.tensor_tensor(out=xt[:, ts, :], in0=xt[:, ts, :],
                                                                        in1=xt[:, xs, :], op=ALU.add),
                     reads=[("xt", ts), ("xt", xs)], writes=[("xt", ts)])
                P.op("sync", lambda e, tt=tt, ts=ts: e.dma_start(out=out_d[b, tt * 128:(tt + 1) * 128, :], in_=xt[:, ts, :]),
                     reads=[("xt", ts)], writes=[("xd", b, tt)], dma=True, semkey="xst%d" % (tt % 2))

        epst = sb("epst", [128, 1], F32)
        P.op("vector", lambda e: e.memset(epst[:], EPS), writes=["epst"])
        P.fence()
        EPS_AP = epst[:, 0:1]

        stage = [0]

        def chk():
            stage[0] += 1
            if stop is not None and stage[0] >= stop:
                raise _Stop()

        def main_loop():
          for b in range(nseq):
            chk()
            rope_tables(b)
            chk()
            for l in range(depth):
                xsrc = x_d if l == 0 else out_d
                load_wslice(l, 0, 0)
                norm_phase(b, l, xsrc)
                chk()
                if dbg and b == 0 and l == 0:
                    dump("hT", hT[:], [("hT", g_) for g_ in range(4)])
                for mixer in range(2):
                    for hf in range(2):
                        proj_phase(b, l, mixer, hf)
                        chk()
                        nm, nh = (mixer, hf + 1) if hf == 0 else (mixer + 1, 0)
                        if nm < 2:
                            load_wslice(l, nm, nh)
                        else:
                            load_wout(l)
                        if dbg and b == 0 and l == 0 and hf == 0:
                            dump("qk%d" % mixer, QK[:], [("QK", s_, t_) for s_ in range(8) for t_ in range(NT)] + [("Vp", r_) for r_ in range(16)])
                            dump("v%d" % mixer, Vt[:], [("V", t_) for t_ in range(NT)] + ["Vones"])
                        if mixer == 0:
                            attn_A(b, l, hf)
                        else:
                            attn_B(b, l, hf)
                        chk()
                if dbg and b == 0 and l == 0:
                    dump("OT", OT[:], [("OT", c_, t_) for c_ in range(8) for t_ in range(4)])
                out_phase(b, l, xsrc)
        try:
            main_loop()
        except _Stop:
            pass
        P.fence()
        P.op("sync", None)
        P.finalize()
        sems = {k: es.enter_context(nc.semaphore("s_%s_%s" % (k[0], k[1]))) for k in P.semkeys}
        with nc.Block() as block:
            P.emit(block, sems)
    return nc


def _consts():
    k = np.arange(128)[:, None]
    q = np.arange(128)[None, :]
    mm = np.zeros((128, 6, 128), np.float32)
    mod4 = ((q - k) % 4 == 0)
    mm[:, 0] = ((q <= k) & mod4)
    mm[:, 1] = mod4
    mm[:, 2] = mod4
    mm[:, 3] = (q <= k).astype(np.float32) + mod4
    mm[:, 4] = (q >= k).astype(np.float32) + ((q >= k) & mod4)
    mm[:, 5] = (q >= k)
    mb = np.where(mm > 1.5, 8.0 * np.log(2.0), np.where(mm > 0.5, 0.0, -BIG)).astype(np.float32)
    m = np.zeros((128, 13, 128), np.float32)
    m[:, 0:6] = mb
    for t_ in range(9, 13):
        m[:, t_] = mb[:, 5]
    invf = (500000.0 ** (-np.arange(0, 16, 2, dtype=np.float32) / 16.0)).astype(np.float32)
    invf = np.broadcast_to(invf[None, :], (128, 8)).copy()
    gb = np.zeros((8, 8), np.float32)
    cb = np.zeros((8, 8), np.float32)
    for b in range(8):
        for n in range(8):
            gb[b, n] = 0.0 if n < b else (1e30 if n == b else -1e30)
            cb[b, n] = -BIG if n > b else 0.0
    gb = np.broadcast_to(np.repeat(gb, 2, axis=0)[None], (128, 16, 8)).copy()
    cb = np.broadcast_to(np.repeat(cb, 2, axis=0)[None], (128, 16, 8)).copy()
    oh = np.zeros((8, 4, S), np.float32)
    for n in range(8):
        oh[n, :, n * 256:(n + 1) * 256] = 1.0
    return m, invf, gb, cb, oh


def make_in_maps(x, c, positions, pre_norm_gain, post_norm_gain, w_ada, b_ada, w_in, w_out, nseq=2, ncores=NCORES):
    m, invf, gb, cb, oh = _consts()
    f = lambda a: np.ascontiguousarray(a, dtype=np.float32)
    pregT = f(pre_norm_gain.reshape(2, 8, 128).transpose(2, 0, 1))
    badaT = f(b_ada[:, :2048].reshape(2, 16, 128).transpose(2, 0, 1))
    badag = f(b_ada[:, 2048:])
    maps = []
    for i in range(ncores):
        sl = slice(i * nseq, (i + 1) * nseq)
        cT = np.zeros((128, 8, 2), np.float32)
        cT[:, :, :nseq] = c[sl].reshape(nseq, 8, 128).transpose(2, 1, 0)
        pos = np.zeros((128, 2, NT), np.int32)
        pos[:, :nseq, :] = positions[sl].reshape(nseq, NT, 128).transpose(2, 0, 1)
        maps.append({
            "x": f(x[sl]), "cT": cT, "pos": pos, "pregT": pregT, "badaT": badaT, "badag": badag,
            "postg": f(post_norm_gain), "w_ada": f(w_ada), "w_in": f(w_in), "w_out": f(w_out),
            "masks": m, "invf": invf, "gbias": gb, "cbias": cb, "onehot": oh,
        })
    return maps


def kernel(x, c, positions, pre_norm_gain, post_norm_gain, w_ada, b_ada, w_in, w_out):
    x = np.asarray(x); c = np.asarray(c); positions = np.asarray(positions)
    maps = make_in_maps(x, c, positions, np.asarray(pre_norm_gain), np.asarray(post_norm_gain),
                        np.asarray(w_ada), np.asarray(b_ada), np.asarray(w_in), np.asarray(w_out))
    nc = build_program(2, 2)
    res = run_bass_kernel_spmd(nc, maps, core_ids=list(range(NCORES)))
    return np.concatenate([np.asarray(r["out"]) for r in res.results], axis=0).astype(np.float32)
```
